# Optimizing a Trainium2 kernel written in Bass

```python
import jax, jax.numpy as jnp
from jax import lax
import numpy as np

D_MODEL = 1024
BATCH = 4
SEQ = 8192
DEPTH = 1

GRID_W = 64
CTX_LEN = 256
GLA_HEADS = 4
GLA_DK = 64
GLA_DV = 128
GLA_WIDTH = GLA_HEADS * GLA_DV
GATE_RANK = 16
GATE_NORMALIZER = 16.0
CHUNK = 64
POOL_WINDOWS = (2, 4, 8, 16)
POOL_GROUP = 128
POOL_WIDTH = POOL_GROUP * len(POOL_WINDOWS)
MIX_WIDTH = GLA_WIDTH + POOL_WIDTH
Q_COLS = GLA_HEADS * GLA_DK
K_COLS = GLA_HEADS * GLA_DK
V_COLS = GLA_WIDTH
G_COLS = GLA_WIDTH
A_COLS = 2 * GATE_RANK
P_COLS = POOL_WIDTH
OFF_K = Q_COLS
OFF_V = OFF_K + K_COLS
OFF_G = OFF_V + V_COLS
OFF_A = OFF_G + G_COLS
OFF_P = OFF_A + A_COLS
IN_COLS = OFF_P + P_COLS
N_GROUPS = 4
EXPERTS_PER_GROUP = 8
N_EXPERTS = N_GROUPS * EXPERTS_PER_GROUP
TOP_K_INNER = 2
D_EXPERT = 256
EPS = 1e-6

kernel_name = "hybrid_gla_pool_hmoe_prefix_dit"


def rmsnorm(x, g):
    xf = x.astype(jnp.float32)
    y = xf * lax.rsqrt(jnp.mean(xf * xf, axis=-1, keepdims=True) + EPS)
    return (y * g).astype(x.dtype)


def modulate(h, shift, scale):
    return h * (1.0 + scale) + shift


def flip(t):
    return jnp.flip(t, axis=1)


def split_heads_qkv(q, k, v):
    B, L, _ = q.shape
    q = q.reshape(B, L, GLA_HEADS, GLA_DK) * (GLA_DK ** -0.5)
    k = k.reshape(B, L, GLA_HEADS, GLA_DK)
    v = v.reshape(B, L, GLA_HEADS, GLA_DV)
    return q, k, v


def log_decay(a_low, w_dec, b_dec):
    B, L, _ = a_low.shape
    a = a_low.reshape(B, L, 2, GATE_RANK).astype(jnp.float32)
    la = jax.nn.log_sigmoid(jnp.einsum('bldr,drk->bldk', a, w_dec) + b_dec) / GATE_NORMALIZER
    la = la.reshape(B, L, 2, GLA_HEADS, GLA_DK)
    return la[:, :, 0], la[:, :, 1]


def gla_state(k, v, la, s0):
    B, L, H, dk = k.shape
    dv = v.shape[-1]
    N = L // CHUNK
    kc = k.reshape(B, N, CHUNK, H, dk).astype(jnp.float32)
    vc = v.reshape(B, N, CHUNK, H, dv)
    b = jnp.cumsum(la.reshape(B, N, CHUNK, H, dk), axis=2)
    b_last = b[:, :, -1]
    k_st = kc * jnp.exp(b_last[:, :, None] - b)
    d_state = jnp.einsum('bnchd,bnche->nbhde', k_st, vc)
    decay = jnp.exp(b_last).transpose(1, 0, 2, 3)

    def step(S, inp):
        d, ds = inp
        return d[..., None] * S + ds, S

    s_fin, s_prev = lax.scan(step, s0, (decay, d_state))
    return s_prev, s_fin, b


def gla_chunked(q, k, v, la, s0):
    s_prev, s_fin, b = gla_state(k, v, la, s0)
    B, L, H, dk = q.shape
    dv = v.shape[-1]
    N = L // CHUNK
    qb = q.reshape(B, N, CHUNK, H, dk).astype(jnp.float32) * jnp.exp(b)
    kb = k.reshape(B, N, CHUNK, H, dk).astype(jnp.float32) * jnp.exp(-b)
    vc = v.reshape(B, N, CHUNK, H, dv)
    mask = jnp.tril(jnp.ones((CHUNK, CHUNK), dtype=bool))
    scores = jnp.where(mask, jnp.einsum('bnihd,bnjhd->bnhij', qb, kb), 0.0)
    o = jnp.einsum('bnhij,bnjhe->bnihe', scores, vc) + jnp.einsum('bnihd,nbhde->bnihe', qb, s_prev)
    return o.reshape(B, L, H, dv), s_fin


def gla_bidirectional(q, k, v, la_f, la_b, s0_f, s0_b):
    o_f, s_f = gla_chunked(q, k, v, la_f, s0_f)
    o_b, s_b = gla_chunked(flip(q), flip(k), flip(v), flip(la_b), s0_b)
    return o_f + flip(o_b), s_f, s_b


def window_mean(x, w, axis):
    n = x.shape[axis]
    lo = w // 2
    hi = w - 1 - lo
    cs = jnp.cumsum(x.astype(jnp.float32), axis=axis)
    pad = [(0, 0)] * x.ndim
    pad[axis] = (1, 0)
    cs = jnp.pad(cs, pad)
    idx = np.arange(n)
    start = np.clip(idx - lo, 0, n)
    end = np.clip(idx + hi + 1, 0, n)
    s = jnp.take(cs, jnp.asarray(end), axis=axis) - jnp.take(cs, jnp.asarray(start), axis=axis)
    cshape = [1] * x.ndim
    cshape[axis] = n
    cnt = jnp.asarray((end - start).astype(np.float32).reshape(cshape))
    return s / cnt


def pool_mixer(p, w_pool, pool_scale, rows):
    B, L, _ = p.shape
    groups = p.reshape(B, L, len(POOL_WINDOWS), POOL_GROUP)
    outs = []
    for gi, w in enumerate(POOL_WINDOWS):
        xg = groups[:, :, gi]
        if rows is None:
            m = window_mean(xg, w, 1)
        else:
            xg2 = xg.reshape(B, rows, GRID_W, POOL_GROUP)
            m = window_mean(window_mean(xg2, w, 2), w, 1).reshape(B, L, POOL_GROUP)
        outs.append(m - xg)
    y = jnp.stack(outs, axis=2)
    y = jnp.einsum('blgc,gcd->blgd', y, w_pool).reshape(B, L, POOL_WIDTH)
    return y * pool_scale


def token_mixer(h, w_in, w_dec, b_dec, gla_norm_g, w_pool, pool_scale, w_out, s0_f, s0_b, rows):
    B, L, _ = h.shape
    proj = h @ w_in
    q, k, v, g, a_low, p = jnp.split(proj, [OFF_K, OFF_V, OFF_G, OFF_A, OFF_P], axis=-1)
    q, k, v = split_heads_qkv(q, k, v)
    la_f, la_b = log_decay(a_low, w_dec, b_dec)
    o, s_f, s_b = gla_bidirectional(q, k, v, la_f, la_b, s0_f, s0_b)
    o = rmsnorm(o, gla_norm_g).reshape(B, L, GLA_WIDTH) * jax.nn.silu(g)
    pooled = pool_mixer(p, w_pool, pool_scale, rows)
    out = jnp.concatenate([o, pooled.astype(o.dtype)], axis=-1) @ w_out
    return out, s_f, s_b


def context_states(h, w_in, w_dec, b_dec):
    B, L, _ = h.shape
    kv = h @ w_in[:, OFF_K:OFF_G]
    a_low = h @ w_in[:, OFF_A:OFF_P]
    k = kv[..., :K_COLS].reshape(B, L, GLA_HEADS, GLA_DK)
    v = kv[..., K_COLS:].reshape(B, L, GLA_HEADS, GLA_DV)
    la_f, la_b = log_decay(a_low, w_dec, b_dec)
    s0 = jnp.zeros((B, GLA_HEADS, GLA_DK, GLA_DV), jnp.float32)
    _, s_f, _ = gla_state(k, v, la_f, s0)
    _, s_b, _ = gla_state(flip(k), flip(v), flip(la_b), s0)
    return s_f, s_b


def hier_moe(h, w_rg, w_re, w_e_in, w_e_out):
    B, L, D = h.shape
    t = h.reshape(-1, D)
    pg = jax.nn.softmax((t @ w_rg).astype(jnp.float32), axis=-1)
    gp, gi = lax.top_k(pg, 1)
    le = (t @ w_re).astype(jnp.float32).reshape(-1, N_GROUPS, EXPERTS_PER_GROUP)
    le = jnp.take_along_axis(le, gi[:, :, None], axis=1)[:, 0]
    ew, ei = lax.top_k(jax.nn.softmax(le, axis=-1), TOP_K_INNER)
    ew = ew / jnp.sum(ew, axis=-1, keepdims=True)
    weights = gp * ew
    eid = gi * EXPERTS_PER_GROUP + ei
    gates = jnp.einsum('tk,tke->et', weights, jax.nn.one_hot(eid, N_EXPERTS, dtype=jnp.float32))

    def body(acc, inp):
        wi, wo, ge = inp
        a, u = jnp.split(t @ wi, 2, axis=-1)
        return acc + ((jax.nn.silu(a) * u) @ wo) * ge[:, None], None

    y, _ = lax.scan(body, jnp.zeros(t.shape, jnp.float32), (w_e_in, w_e_out, gates))
    return y.reshape(B, L, D).astype(h.dtype)


def setup_inputs(seed: int = 0) -> dict:
    key = jax.random.key(seed)
    ks = jax.random.split(key, 24)
    f32 = jnp.float32

    def nrm(k, shape, s):
        return jax.random.normal(k, shape, f32) * s

    D = D_MODEL
    return {
        "x": nrm(ks[0], (BATCH, SEQ, D), 1.0),
        "c": nrm(ks[1], (BATCH, D), 1.0),
        "ctx": nrm(ks[2], (BATCH, CTX_LEN, D), 1.0),
        "c_ctx": nrm(ks[3], (D,), 1.0),
        "w_ada": nrm(ks[4], (DEPTH, D, 6 * D), D ** -0.5),
        "b_ada": nrm(ks[5], (DEPTH, 6 * D), 0.02),
        "norm1_g": 1.0 + nrm(ks[6], (DEPTH, D), 0.02),
        "w_in": nrm(ks[7], (DEPTH, D, IN_COLS), D ** -0.5),
        "w_decay": nrm(ks[8], (DEPTH, 2, GATE_RANK, GLA_HEADS * GLA_DK), GATE_RANK ** -0.5),
        "b_decay": nrm(ks[9], (DEPTH, 2, GLA_HEADS * GLA_DK), 0.1),
        "gla_norm_g": 1.0 + nrm(ks[10], (DEPTH, GLA_DV), 0.02),
        "w_pool": nrm(ks[11], (DEPTH, len(POOL_WINDOWS), POOL_GROUP, POOL_GROUP), POOL_GROUP ** -0.5),
        "pool_scale": 1.0 + nrm(ks[12], (DEPTH, POOL_WIDTH), 0.02),
        "w_out": nrm(ks[13], (DEPTH, MIX_WIDTH, D), MIX_WIDTH ** -0.5),
        "norm2_g": 1.0 + nrm(ks[14], (DEPTH, D), 0.02),
        "w_router_group": nrm(ks[15], (DEPTH, D, N_GROUPS), D ** -0.5),
        "w_router_expert": nrm(ks[16], (DEPTH, D, N_EXPERTS), D ** -0.5),
        "w_expert_in": nrm(ks[17], (DEPTH, N_EXPERTS, D, 2 * D_EXPERT), D ** -0.5),
        "w_expert_out": nrm(ks[18], (DEPTH, N_EXPERTS, D_EXPERT, D), D_EXPERT ** -0.5),
        "final_norm_g": 1.0 + nrm(ks[19], (D,), 0.02),
    }


def reference(x, c, ctx, c_ctx, w_ada, b_ada, norm1_g, w_in, w_decay, b_decay, gla_norm_g, w_pool,
              pool_scale, w_out, norm2_g, w_router_group, w_router_expert, w_expert_in, w_expert_out,
              final_norm_g):
    rows = x.shape[1] // GRID_W
    xc = ctx
    for i in range(DEPTH):
        mod = jax.nn.silu(c) @ w_ada[i] + b_ada[i]
        sh_a, sc_a, gt_a, sh_m, sc_m, gt_m = jnp.split(mod[:, None, :], 6, axis=-1)
        mod_c = jax.nn.silu(c_ctx) @ w_ada[i] + b_ada[i]
        csh_a, csc_a, cgt_a, csh_m, csc_m, cgt_m = jnp.split(mod_c, 6, axis=-1)

        hc = modulate(rmsnorm(xc, norm1_g[i]), csh_a, csc_a)
        if i + 1 < DEPTH:
            zero_s = jnp.zeros((xc.shape[0], GLA_HEADS, GLA_DK, GLA_DV), jnp.float32)
            out_c, s_f, s_b = token_mixer(hc, w_in[i], w_decay[i], b_decay[i], gla_norm_g[i], w_pool[i],
                                          pool_scale[i], w_out[i], zero_s, zero_s, None)
            xc_next = xc + (cgt_a * out_c).astype(xc.dtype)
            hc2 = modulate(rmsnorm(xc_next, norm2_g[i]), csh_m, csc_m)
            xc_next = xc_next + (cgt_m * hier_moe(hc2, w_router_group[i], w_router_expert[i],
                                                  w_expert_in[i], w_expert_out[i])).astype(xc.dtype)
        else:
            s_f, s_b = context_states(hc, w_in[i], w_decay[i], b_decay[i])
            xc_next = xc

        h = modulate(rmsnorm(x, norm1_g[i]), sh_a, sc_a)
        out, _, _ = token_mixer(h, w_in[i], w_decay[i], b_decay[i], gla_norm_g[i], w_pool[i],
                                pool_scale[i], w_out[i], s_f, s_b, rows)
        x = x + (gt_a * out).astype(x.dtype)
        h2 = modulate(rmsnorm(x, norm2_g[i]), sh_m, sc_m)
        x = x + (gt_m * hier_moe(h2, w_router_group[i], w_router_expert[i],
                                 w_expert_in[i], w_expert_out[i])).astype(x.dtype)
        xc = xc_next
    return rmsnorm(x, final_norm_g)
```

```python
import numpy as np
import concourse.bass as bass
import concourse.mybir as mybir
from concourse.bass_utils import run_bass_kernel_spmd

F32 = mybir.dt.float32
BF16 = mybir.dt.bfloat16
AF = mybir.ActivationFunctionType
ALU = mybir.AluOpType
AX = mybir.AxisListType


class Buf:
    __slots__ = ("name", "last_w", "readers")

    def __init__(self, name):
        self.name = name
        self.last_w = None
        self.readers = []


class DSem:
    __slots__ = ("sem", "count", "open_ops")

    def __init__(self, sem):
        self.sem = sem
        self.count = 0
        self.open_ops = []


class Op:
    __slots__ = ("eng", "fn", "deps", "signal", "semval", "dsem", "is_dma", "idx")


class Prog:
    ENGS = ("pe", "act", "dve", "pool", "sp")

    def __init__(self, nc):
        self.nc = nc
        self.ops = []
        self.by_eng = {e: [] for e in self.ENGS}

    def op(self, eng, fn, reads=(), writes=(), dsem=None, batch=False):
        o = Op()
        o.eng = eng
        o.fn = fn
        o.deps = []
        o.signal = False
        o.semval = None
        o.dsem = dsem
        o.is_dma = dsem is not None
        o.idx = len(self.ops)
        if o.is_dma:
            dsem.count += 16
            o.semval = dsem.count
            o.signal = True
            if batch:
                for q in dsem.open_ops:
                    q.semval = dsem.count
            else:
                dsem.open_ops = []
            dsem.open_ops.append(o)
        deps = set()
        for b in reads:
            if b.last_w is not None:
                deps.add(b.last_w)
        for b in writes:
            if b.last_w is not None:
                deps.add(b.last_w)
            for r in b.readers:
                deps.add(r)
        same_batch = set(id(q) for q in dsem.open_ops) if (o.is_dma and batch) else ()
        for p in deps:
            if p is o or id(p) in same_batch:
                continue
            if p.idx < getattr(self, "n_emitted_ops", 0):
                continue
            if (not p.is_dma) and (not o.is_dma) and p.eng == eng:
                if eng == "pe":
                    continue
                raw = any(b.last_w is p for b in reads) or any(b.last_w is p for b in writes)
                if not raw:
                    continue
            o.deps.append(p)
            p.signal = True
        for b in reads:
            b.readers.append(o)
        for b in writes:
            b.last_w = o
            b.readers = []
        self.ops.append(o)
        self.by_eng[eng].append(o)
        return o

    def emit(self, block, sems, final=()):
        self.sems = sems
        self.bar = None
        self.emit_phase(block, final=final)

    def emit_phase(self, block, final=()):
        sems = self.sems
        if not hasattr(self, "cnt"):
            self.cnt = {e: 0 for e in self.ENGS}
            self.waited = {e: {} for e in self.ENGS}
            self.emitted = {e: 0 for e in self.ENGS}
            self.phase = 0
        self.phase += 1
        self.n_emitted_ops = len(self.ops)
        streams = {}
        for e in self.ENGS:
            streams[e] = self.by_eng[e][self.emitted[e]:]
            self.emitted[e] = len(self.by_eng[e])
            c = self.cnt[e]
            for o in streams[e]:
                if (not o.is_dma) and o.signal:
                    c += 1
                    o.semval = c
            self.cnt[e] = c
        prog = self
        phase = self.phase

        def run(engname, engine):
            waited = prog.waited[engname]
            my_dsems = {}
            for o in streams[engname]:
                for p in o.deps:
                    if p.is_dma:
                        s, v = p.dsem.sem, p.semval
                    else:
                        s, v = sems[p.eng], p.semval
                    key = s.num
                    if waited.get(key, 0) >= v:
                        continue
                    waited[key] = v
                    engine.wait_ge(s, v)
                ins = o.fn(engine)
                if o.is_dma:
                    ins.then_inc(o.dsem.sem, 16)
                    my_dsems[o.dsem.sem.num] = (o.dsem.sem, o.semval)
                elif o.signal:
                    ins.then_inc(sems[engname], 1)
            for s, v in my_dsems.values():
                if waited.get(s.num, 0) < v:
                    waited[s.num] = v
                    engine.wait_ge(s, v)
            if engname == "sp":
                for d in final:
                    engine.wait_ge(d.sem, d.count)
            if prog.bar is not None:
                engine.sem_inc(prog.bar, 1)
                engine.wait_ge(prog.bar, 5 * phase)

        @block.tensor
        def _(eng):
            run("pe", eng)

        @block.scalar
        def _(eng):
            run("act", eng)

        @block.vector
        def _(eng):
            run("dve", eng)

        @block.gpsimd
        def _(eng):
            run("pool", eng)

        @block.sync
        def _(eng):
            run("sp", eng)

    def mm(self, out, lhsT, rhs, start=True, stop=True, reads=(), writes=()):
        return self.op("pe", lambda e: e.matmul(out, lhsT, rhs, start=start, stop=stop), reads, writes)

    def tr(self, out, in_, ident, reads=(), writes=()):
        return self.op("pe", lambda e: e.transpose(out, in_, ident), reads, writes)

    def act(self, out, in_, func, reads=(), writes=(), **kw):
        return self.op("act", lambda e: e.activation(out, in_, func, **kw), reads, writes)

    def tt(self, eng, out, in0, in1, op, reads=(), writes=()):
        return self.op(eng, lambda e: e.tensor_tensor(out, in0, in1, op), reads, writes)

    def ts(self, eng, out, in0, s1, s2, op0, op1=None, reads=(), writes=()):
        if op1 is None:
            return self.op(eng, lambda e: e.tensor_scalar(out, in0, s1, s2, op0), reads, writes)
        return self.op(eng, lambda e: e.tensor_scalar(out, in0, s1, s2, op0, op1), reads, writes)

    def stt(self, eng, out, in0, scalar, in1, op0, op1, reads=(), writes=()):
        return self.op(eng, lambda e: e.scalar_tensor_tensor(out, in0, scalar, in1, op0, op1), reads, writes)

    def cp(self, eng, out, in_, reads=(), writes=()):
        if eng == "act":
            return self.op(eng, lambda e: e.copy(out, in_), reads, writes)
        return self.op(eng, lambda e: e.tensor_copy(out, in_), reads, writes)

    def dma(self, eng, out, in_, dsem, reads=(), writes=(), batch=False):
        return self.op(eng, lambda e: e.dma_start(out=out, in_=in_), reads, writes, dsem=dsem, batch=batch)

    def reset_tracking(self, bufs):
        for b in bufs:
            b.last_w = None
            b.readers = []


class TB:
    __slots__ = ("t", "b")

    def __init__(self, t, name):
        self.t = t
        self.b = Buf(name)


class Ring:
    def __init__(self, items):
        self.items = items
        self.i = -1

    def next(self):
        self.i = (self.i + 1) % len(self.items)
        return self.items[self.i]

    def cur(self):
        return self.items[self.i]


D = 1024
KC = 8
GRID_W = 64
IN_COLS = 2080
OFF_Q, OFF_K, OFF_V, OFF_G, OFF_A, OFF_P = 0, 256, 512, 1024, 1536, 1568
NEXP = 32
DEXP = 256
EPS = 1e-6
POOL_W = (2, 4, 8, 16)
POOL_R = (1, 1, 2, 4)
HALO = 4
BIG = 1.0e30


def pool_block_table():
    tab = {}
    n = 0
    for gi, R in enumerate(POOL_R):
        for v in range(R + 1):
            for dt in range(-R, R + 1):
                if v < R and v + dt < 0:
                    continue
                tab[(v, gi, dt)] = n
                n += 1
    return tab, n


POOL_TAB, NBLK_A = pool_block_table()


_STOP = 0


def build_program(NT_OWN, NT_OTH, NCTX):
    from contextlib import ExitStack
    NT_ALL = NT_OWN + NT_OTH
    NTOK = NT_OWN * 128
    TPB = min(16, NT_OWN)
    NBLK = NT_OWN // TPB
    nc = bass.Bass("TRN2", target_bir_lowering=False)

    def din(name, shape, dt=F32):
        return nc.dram_tensor(name, list(shape), dt, kind="ExternalInput").ap()

    xs = din("xs", [NT_ALL * 128, D])
    ctxs = din("ctxs", [NCTX * 128, D])
    cvec = din("cvec", [128, KC, 2])
    w_ada = din("w_ada", [D, 6 * D])
    b_adaT = din("b_adaT", [128, 48])
    b_gtB = din("b_gtB", [128, 2, D])
    g12T = din("g12T", [128, 2, KC])
    w_in = din("w_in", [D, IN_COLS])
    wdec = din("wdec", [33, 512])
    ggB_d = din("ggB", [128, 128])
    w_pool = din("w_pool", [4, 128, 128])
    pscT = din("pscT", [128, 4])
    w_out = din("w_out", [D, D])
    w_r = din("w_r", [D, 36])
    w_ei = din("w_ei", [NEXP, D, 2 * DEXP])
    w_eo = din("w_eo", [NEXP, DEXP, D])
    fgB_d = din("fgB", [128, D])
    cmat = din("cmat", [3, 128, 128])
    apool = din("apool", [NBLK_A, 128, 128])
    y = nc.dram_tensor("y", [NTOK, D], F32, kind="ExternalOutput").ap()
    hT_scr = nc.dram_tensor("hT_scr", [NT_OWN, 128, KC, 128], BF16).ap()
    pl_scr = nc.dram_tensor("pl_scr", [NT_OWN, 128, 4, 128], BF16).ap()
    S_scr = nc.dram_tensor("S_scr", [NT_OWN, 128, 2, 128], BF16).ap()
    h2_scr = nc.dram_tensor("h2_scr", [128, KC, NTOK], BF16).ap()

    ges = ExitStack()
    with ges:
        def gsb(name, shape, dt):
            return TB(ges.enter_context(nc.sbuf_tensor("sg_" + name, list(shape), dt)), name)

        def gsem(name):
            return ges.enter_context(nc.semaphore(name))

        P = Prog(nc)
        P.sems = {e: gsem("s_" + e) for e in Prog.ENGS}
        P.bar = gsem("bar")
        dsem_pool = [DSem(gsem("d%d" % i)) for i in range(70)]
        dsem_i = [0]

        def new_dsem():
            d = dsem_pool[dsem_i[0]]
            dsem_i[0] += 1
            return d

        hT_b = [Buf("hTs%d" % i) for i in range(NT_OWN)]
        pl_b = [Buf("pls%d" % i) for i in range(NT_OWN)]
        S_b = [Buf("Ss%d" % i) for i in range(NT_OWN)]
        h2_b = [Buf("h2s%d" % i) for i in range(NT_OWN)]
        y_b = [Buf("ys%d" % i) for i in range(NT_OWN)]

        ident = gsb("ident", [128, 128], BF16)
        triF = gsb("triF", [128, 128], BF16)
        triB = gsb("triB", [128, 128], BF16)
        tri = (triF, triB)
        modv = gsb("modv", [128, 6, KC], F32)
        gtB = gsb("gtB", [128, 2, D], F32)
        fgB = gsb("fgB", [128, D], F32)
        ggB = gsb("ggB", [128, 128], F32)
        psc = gsb("psc", [128, 4], F32)
        gates = gsb("gates", [128, NT_OWN, NEXP], F32)
        dconst = new_dsem()
        dconst_p = new_dsem()
        for tbv, src in ((ident, cmat[0]), (triF, cmat[1]), (triB, cmat[2])):
            P.dma("pool", tbv.t[:], src, dconst_p, writes=[tbv.b], batch=True)
        P.dma("sp", fgB.t[:], fgB_d, dconst, writes=[fgB.b], batch=True)
        P.dma("sp", ggB.t[:], ggB_d, dconst, writes=[ggB.b], batch=True)
        P.dma("sp", psc.t[:], pscT, dconst, writes=[psc.b], batch=True)

        wes = ExitStack()
        w_in_sb = TB(wes.enter_context(nc.sbuf_tensor("sg_w_in_sb", [128, KC, IN_COLS], BF16)), "w_in_sb")
        wdec_sb = TB(wes.enter_context(nc.sbuf_tensor("sg_wdec_sb", [33, 512], BF16)), "wdec_sb")
        dw = new_dsem()
        for kc in range(KC):
            P.dma("pool", w_in_sb.t[:, kc, :], w_in[kc * 128:(kc + 1) * 128, :], dw, writes=[w_in_sb.b], batch=True)
        P.dma("pool", wdec_sb.t[:], wdec, dw, writes=[wdec_sb.b], batch=True)

        with ExitStack() as es:
            def sb(name, shape, dt):
                return TB(es.enter_context(nc.sbuf_tensor("sb_" + name, list(shape), dt)), name)

            def ps(name, shape, dt=F32):
                return TB(es.enter_context(nc.psum_tensor("ps_" + name, list(shape), dt)), name)

            cv = sb("cv", [128, KC, 2], F32)
            sv = sb("sv", [128, KC, 2], F32)
            srep = sb("srep", [128, KC, 128], F32)
            wst = Ring([sb("wst%d" % i, [128, KC, 512], F32) for i in range(3)])
            wst_d = [new_dsem(), new_dsem(), new_dsem()]
            wst_q = ("sp", "act", "sp")
            bT = sb("bT", [128, 48], F32)
            bgt = sb("bgt", [128, 2, D], F32)
            g12 = sb("g12", [128, 2, KC], F32)
            mv = sb("mv", [128, 4, KC, 2], F32)
            psg = Ring([ps("psg%d" % i, [128, 512]) for i in range(2)])
            psv = Ring([ps("psv%d" % i, [128, 8]) for i in range(2)])
            P.dma("sp", cv.t[:], cvec, dconst, writes=[cv.b], batch=True)
            P.dma("sp", bT.t[:], b_adaT, dconst, writes=[bT.b], batch=True)
            P.dma("sp", bgt.t[:], b_gtB, dconst, writes=[bgt.b], batch=True)
            P.dma("sp", g12.t[:], g12T, dconst, writes=[g12.b], batch=True)
            P.act(sv.t[:], cv.t[:], AF.Silu, reads=[cv.b], writes=[sv.b])
            P.cp("dve", srep.t[:], sv.t[:, :, 0:1].to_broadcast([128, KC, 128]), reads=[sv.b], writes=[srep.b])
            vec_of_block = {0: 0, 1: 0, 2: 1, 3: 1, 6: 2, 7: 2, 8: 3, 9: 3}
            for j in range(12):
                w = wst.next()
                P.dma(wst_q[wst.i], w.t[:], w_ada[:, j * 512:(j + 1) * 512].rearrange("(k p) c -> p k c", p=128),
                      wst_d[wst.i], writes=[w.b])
                half = j % 2
                if j in (4, 5, 10, 11):
                    gi = 0 if j < 6 else 1
                    pt = psg.next()
                    for kc in range(KC):
                        P.mm(pt.t[:, :], srep.t[:, kc, :], w.t[:, kc, :], start=(kc == 0), stop=(kc == KC - 1),
                             reads=[srep.b, w.b], writes=[pt.b])
                    P.tt("dve", gtB.t[:, gi, half * 512:(half + 1) * 512], pt.t[:, :],
                         bgt.t[:, gi, half * 512:(half + 1) * 512], ALU.add, reads=[pt.b, bgt.b], writes=[gtB.b])
                else:
                    vi = vec_of_block[j]
                    pt = psv.next()
                    for cc in range(4):
                        for kc in range(KC):
                            P.mm(pt.t[:, cc * 2:cc * 2 + 2], w.t[:, kc, cc * 128:(cc + 1) * 128], sv.t[:, kc, :],
                                 start=(kc == 0), stop=(kc == KC - 1), reads=[w.b, sv.b], writes=[pt.b])
                    P.tt("dve", mv.t[:, vi, half * 4:half * 4 + 4, :],
                         pt.t[:, :].rearrange("p (a b) -> p a b", b=2),
                         bT.t[:, j * 4:j * 4 + 4].unsqueeze(2).to_broadcast([128, 4, 2]), ALU.add,
                         reads=[pt.b, bT.b], writes=[mv.b])
            P.stt("dve", modv.t[:, 0, :], mv.t[:, 1, :, 0], 1.0, g12.t[:, 0, :], ALU.add, ALU.mult,
                  reads=[mv.b, g12.b], writes=[modv.b])
            P.cp("dve", modv.t[:, 1, :], mv.t[:, 0, :, 0], reads=[mv.b], writes=[modv.b])
            P.stt("dve", modv.t[:, 2, :], mv.t[:, 1, :, 1], 1.0, g12.t[:, 0, :], ALU.add, ALU.mult,
                  reads=[mv.b, g12.b], writes=[modv.b])
            P.cp("dve", modv.t[:, 3, :], mv.t[:, 0, :, 1], reads=[mv.b], writes=[modv.b])
            P.stt("dve", modv.t[:, 4, :], mv.t[:, 3, :, 0], 1.0, g12.t[:, 1, :], ALU.add, ALU.mult,
                  reads=[mv.b, g12.b], writes=[modv.b])
            P.cp("dve", modv.t[:, 5, :], mv.t[:, 2, :, 0], reads=[mv.b], writes=[modv.b])
            with nc.Block() as block:
                P.emit_phase(block)
        if _STOP == 1:
            return nc

        def run_pipeline(tiles, order):
            K = len(order)
            N = len(tiles)
            for i in range(N + K - 1):
                for s in order:
                    t = i - s
                    if 0 <= t < N and tiles[t][s] is not None:
                        tiles[t][s]()

        with ExitStack() as es:
            def sb(name, shape, dt):
                return TB(es.enter_context(nc.sbuf_tensor("sb_" + name, list(shape), dt)), name)

            def ps(name, shape, dt=F32):
                return TB(es.enter_context(nc.psum_tensor("ps_" + name, list(shape), dt)), name)

            def ring(name, n, shape, dt):
                return Ring([sb("%s%d" % (name, i), shape, dt) for i in range(n)])

            def dring(n):
                return [new_dsem() for _ in range(n)]


            xt_r = ring("xt", 3, [128, D], F32)
            xt_d = dring(3)
            sqj = sb("sqj", [128, D], BF16)
            stat_r = ring("stat", 4, [128, 4], F32)
            xn_r = ring("xn", 2, [128, D], BF16)
            mtx_r = ring("mtx", 2, [128, D], F32)
            hT_r = ring("hT", 3, [128, KC, 128], BF16)
            hT_d = dring(3)
            hTs_d = dring(3)
            qk_r = ring("qk", 4, [128, 512], F32)
            v_r = ring("v_sb", 7, [128, 512], BF16)
            a_r = ring("a_sb", 3, [33, 128], BF16)
            e1_r = ring("e1", 2, [128, 512], F32)
            l_r = ring("l_bf", 2, [128, 512], BF16)
            Ek_r = ring("Ek", 3, [128, 512], F32)
            el_r = ring("elast", 6, [128, 4], F32)
            kst_r = ring("kst", 2, [128, 2, 128], BF16)
            kstm_r = ring("kstm", 3, [128, 2, 128], BF16)
            Sb = sb("Sb", [128, 2, 128], F32)
            Sf = sb("Sf", [128, 2, 128], F32)
            Sbbf_r = ring("Sbbf", 5, [128, 2, 128], BF16)
            Sbbf_d = dring(5)
            Sbbfs_d = dring(5)
            Sfbf_r = ring("Sfbf", 3, [128, 2, 128], BF16)
            B0 = ps("B0", [128, 1024], BF16)
            B1 = ps("B1", [128, 512])
            B2 = ps("B2", [128, 512])
            B3 = ps("B3", [128, 512])
            B4 = ps("B4", [128, 512])
            B5 = ps("B5", [128, 512])
            B6 = ps("B6", [128, 512])
            B7a = ps("B7a", [128, 512])
            B7b = B0

            for r_ in a_r.items:
                P.op("dve", lambda e, r_=r_: e.memset(r_.t[:], 1.0), writes=[r_.b])
            P.op("dve", lambda e: e.memset(Sb.t[:], 0.0), writes=[Sb.b])
            P.op("dve", lambda e: e.memset(Sf.t[:], 0.0), writes=[Sf.b])
            for r_ in Sfbf_r.items + Sbbf_r.items:
                P.op("dve", lambda e, r_=r_: e.memset(r_.t[:], 0.0), writes=[r_.b])

            class CX:
                pass

            def load_x(src_ap):
                xt = xt_r.next()
                P.dma("sp", xt.t[:], src_ap, xt_d[xt_r.i], writes=[xt.b])
                return xt

            def norm_act(xt):
                st = stat_r.next()
                P.act(sqj.t[:], xt.t[:], AF.Square, accum_out=st.t[:, 0:1], reads=[xt.b], writes=[st.b])
                P.act(st.t[:, 1:2], st.t[:, 0:1], AF.Ln, bias=EPS, scale=1.0 / D, reads=[st.b], writes=[st.b])
                P.act(st.t[:, 2:3], st.t[:, 1:2], AF.Exp, scale=-0.5, reads=[st.b], writes=[st.b])
                return st

            def norm_dve(xt, st):
                xn = xn_r.next()
                P.act(xn.t[:], xt.t[:], AF.Copy, scale=st.t[:, 2:3], reads=[xt.b, st.b], writes=[xn.b])
                return xn

            def transpose_mod(xn, gi, hT):
                for kc in range(KC):
                    P.tr(B0.t[:, kc * 128:(kc + 1) * 128], xn.t[:, kc * 128:(kc + 1) * 128], ident.t[:],
                         reads=[xn.b, ident.b], writes=[B0.b])
                mx = mtx_r.next()
                P.tt("dve", mx.t[:].rearrange("p (k t) -> p k t", k=KC), B0.t[:, :].rearrange("p (k t) -> p k t", k=KC),
                     modv.t[:, gi, :].unsqueeze(2).to_broadcast([128, KC, 128]), ALU.mult,
                     reads=[B0.b, modv.b], writes=[mx.b])
                P.tt("pool", hT.t[:], mx.t[:].rearrange("p (k t) -> p k t", k=KC),
                     modv.t[:, gi + 1, :].unsqueeze(2).to_broadcast([128, KC, 128]), ALU.add,
                     reads=[mx.b, modv.b], writes=[hT.b])

            def proj_fm(hT, col0, ncol, out_ap, out_b):
                for kc in range(KC):
                    P.mm(out_ap, w_in_sb.t[:, kc, col0:col0 + ncol], hT.t[:, kc, :], start=(kc == 0),
                         stop=(kc == KC - 1), reads=[w_in_sb.b, hT.b], writes=[out_b])

            def proj_tm(hT, col0, out_ap, out_b):
                for kc in range(KC):
                    P.mm(out_ap, hT.t[:, kc, :], w_in_sb.t[:, kc, col0:col0 + 512], start=(kc == 0),
                         stop=(kc == KC - 1), reads=[w_in_sb.b, hT.b], writes=[out_b])

            def decay_mm(a, dirs, PB):
                P.mm(PB.t[:, :], a.t[0:33, :], wdec_sb.t[0:33, :], reads=[a.b, wdec_sb.b], writes=[PB.b])
                e1 = e1_r.next()
                lo, hi = (0, 512) if len(dirs) == 2 else (dirs[0] * 256, dirs[0] * 256 + 256)
                P.act(e1.t[:, lo:hi], PB.t[:, lo:hi], AF.Exp, scale=-1.0, reads=[PB.b], writes=[e1.b])
                l = l_r.next()
                P.act(l.t[:, lo:hi], e1.t[:, lo:hi], AF.Ln, bias=1.0, reads=[e1.b], writes=[l.b])
                return l, lo, hi

            def cumsum_mm(l, dirs):
                for d in dirs:
                    for c in range(2):
                        j = d * 2 + c
                        P.mm(B3.t[:, j * 128:(j + 1) * 128], l.t[:, j * 128:(j + 1) * 128], tri[d].t[:],
                             reads=[l.b, tri[d].b], writes=[B3.b])

            def kst_make(d, Ek, ksrc, k0):
                el = el_r.next()
                last = 127 if d == 0 else 0
                cs3 = B3.t[:, :].rearrange("p (j t) -> p j t", t=128)
                P.act(el.t[:, 0:2], cs3[:, 2 * d:2 * d + 2, last], AF.Exp, scale=-1.0 / 16, reads=[B3.b], writes=[el.b])
                kst = kst_r.next()
                for c in range(2):
                    P.stt("dve", kst.t[:, c, :], ksrc.t[:, k0 + c * 128:k0 + (c + 1) * 128], el.t[:, c:c + 1],
                          Ek.t[:, (d * 2 + c) * 128:(d * 2 + c + 1) * 128], ALU.mult, ALU.mult,
                          reads=[ksrc.b, el.b, Ek.b], writes=[kst.b])
                return el, kst

            def kst_transpose(kst, PB):
                for c in range(2):
                    P.tr(PB.t[:, c * 128:(c + 1) * 128], kst.t[:, c, :], ident.t[:], reads=[kst.b, ident.b],
                         writes=[PB.b])
                kstm = kstm_r.next()
                P.cp("act", kstm.t[:].rearrange("p c t -> p (c t)"), PB.t[:, 0:256], reads=[PB.b], writes=[kstm.b])
                return kstm

            def state_apply(el, kstm, v, S, Sbf_ring, PB):
                for c in range(2):
                    P.mm(PB.t[:, c * 256:(c + 1) * 256], kstm.t[:, c, :], v.t[:, c * 256:(c + 1) * 256],
                         reads=[kstm.b, v.b], writes=[PB.b])
                for c in range(2):
                    for hf in range(2):
                        r0 = hf * 64
                        P.stt("dve", S.t[r0:r0 + 64, c, :], S.t[r0:r0 + 64, c, :], el.t[r0:r0 + 64, c:c + 1],
                              PB.t[r0:r0 + 64, c * 256 + hf * 128:c * 256 + (hf + 1) * 128], ALU.mult, ALU.add,
                              reads=[S.b, el.b, PB.b], writes=[S.b])
                sbf = Sbf_ring.next()
                P.cp("pool", sbf.t[:], S.t[:], reads=[S.b], writes=[sbf.b])
                return sbf

            with ExitStack() as esa:
                def sba(name, shape, dt):
                    return TB(esa.enter_context(nc.sbuf_tensor("sa_" + name, list(shape), dt)), name)

                NP = 14
                p_r = [sba("p_sb%d" % i, [128, 512], BF16) for i in range(NP)]
                diff_r = Ring([sba("diff%d" % i, [128, 4, 128], BF16) for i in range(2)])
                pl_r = Ring([sba("plbf%d" % i, [128, 4, 128], BF16) for i in range(2)])
                pl_d = dring(2)
                wpool_sb = sba("wpool_sb", [128, 4, 128], BF16)
                A_sb = sba("A_sb", [128, NBLK_A, 128], BF16)
                dw2 = new_dsem()
                P.dma("pool", wpool_sb.t[:], w_pool.rearrange("g c d -> c g d"), dw2, writes=[wpool_sb.b], batch=True)
                for b0 in range(0, NBLK_A, 4):
                    b1 = min(NBLK_A, b0 + 4)
                    P.dma("pool", A_sb.t[:, b0:b1, :], apool[b0:b1].rearrange("n k t -> k n t"), dw2, writes=[A_sb.b],
                          batch=True)

                def slot_of(tile):
                    return tile % NP

                def pool_fin1(tt):
                    for gi, R in enumerate(POOL_R):
                        v_ = tt if tt < R else R
                        dts = [dt for dt in range(-R, R + 1) if tt + dt >= 0]
                        for n_, dt in enumerate(dts):
                            pt = p_r[slot_of(tt + dt)]
                            blk = POOL_TAB[(v_, gi, dt)]
                            P.mm(B2.t[:, gi * 128:(gi + 1) * 128], pt.t[:, gi * 128:(gi + 1) * 128], A_sb.t[:, blk, :],
                                 start=(n_ == 0), stop=(n_ == len(dts) - 1), reads=[pt.b, A_sb.b], writes=[B2.b])
                    df = diff_r.next()
                    P.cp("act", df.t[:].rearrange("p g t -> p (g t)"), B2.t[:, :], reads=[B2.b], writes=[df.b])
                    return df

                def pool_fin2(tt, df):
                    for gi in range(4):
                        P.mm(B6.t[:, gi * 128:(gi + 1) * 128], wpool_sb.t[:, gi, :], df.t[:, gi, :],
                             reads=[wpool_sb.b, df.b], writes=[B6.b])
                    pl = pl_r.next()
                    P.tt("dve", pl.t[:], B6.t[:, :].rearrange("p (g t) -> p g t", g=4),
                         psc.t[:, :].unsqueeze(2).to_broadcast([128, 4, 128]), ALU.mult, reads=[B6.b, psc.b], writes=[pl.b])
                    P.dma("pool", pl_scr[tt], pl.t[:], pl_d[pl_r.i], reads=[pl.b], writes=[pl_b[tt]])

                def a_tile(src_ap, gi, d, S, Sring, own_tau, pool_tau, fin_tt):
                    cx = CX()

                    def s0():
                        cx.xt = load_x(src_ap)
                        cx.st = norm_act(cx.xt)

                    def s1():
                        cx.xn = norm_dve(cx.xt, cx.st)

                    def s2():
                        cx.hT = hT_r.next()
                        transpose_mod(cx.xn, gi, cx.hT)
                        if own_tau is not None:
                            P.dma("pool", hT_scr[own_tau], cx.hT.t[:], hTs_d[hT_r.i], reads=[cx.hT.b],
                                  writes=[hT_b[own_tau]])

                    def s3():
                        hT = cx.hT
                        proj_fm(hT, OFF_K, 128, B1.t[:, 0:128], B1.b)
                        proj_fm(hT, OFF_K + 128, 128, B1.t[:, 128:256], B1.b)
                        proj_tm(hT, OFF_V, B2.t[:, :], B2.b)
                        proj_fm(hT, OFF_A, 32, B7a.t[0:32, 0:128], B7a.b)
                        cx.kq = qk_r.next()
                        P.cp("dve", cx.kq.t[:, 0:256], B1.t[:, 0:256], reads=[B1.b], writes=[cx.kq.b])
                        cx.v = v_r.next()
                        P.cp("act", cx.v.t[:], B2.t[:, :], reads=[B2.b], writes=[cx.v.b])
                        cx.a = a_r.next()
                        P.cp("act", cx.a.t[0:32, :], B7a.t[0:32, 0:128], reads=[B7a.b], writes=[cx.a.b])
                        if pool_tau is not None:
                            proj_tm(hT, OFF_P, B6.t[:, :], B6.b)
                            pt = p_r[slot_of(pool_tau)]
                            P.cp("dve", pt.t[:], B6.t[:, :], reads=[B6.b], writes=[pt.b])

                    def s4():
                        cx.l, cx.lo, cx.hi = decay_mm(cx.a, [d], B4)

                    def s5():
                        cumsum_mm(cx.l, [d])
                        Ek = cx.Ek = Ek_r.next()
                        P.act(Ek.t[:, cx.lo:cx.hi], B3.t[:, cx.lo:cx.hi], AF.Exp, scale=1.0 / 16, reads=[B3.b], writes=[Ek.b])
                        el = cx.el = el_r.next()
                        last = 127 if d == 0 else 0
                        cs3 = B3.t[:, :].rearrange("p (j t) -> p j t", t=128)
                        P.act(el.t[:, 0:2], cs3[:, 2 * d:2 * d + 2, last], AF.Exp, scale=-1.0 / 16, reads=[B3.b], writes=[el.b])

                    def s6():
                        kst = cx.kst = kst_r.next()
                        for c in range(2):
                            P.stt("dve", kst.t[:, c, :], cx.kq.t[:, c * 128:(c + 1) * 128], cx.el.t[:, c:c + 1],
                                  cx.Ek.t[:, (d * 2 + c) * 128:(d * 2 + c + 1) * 128], ALU.mult, ALU.mult,
                                  reads=[cx.kq.b, cx.el.b, cx.Ek.b], writes=[kst.b])

                    def s7():
                        cx.kstm = kst_transpose(cx.kst, B7b)

                    def s8():
                        if own_tau is not None:
                            sbf = Sring.cur()
                            P.dma("pool", S_scr[own_tau], sbf.t[:], Sbbfs_d[Sring.i], reads=[sbf.b], writes=[S_b[own_tau]])
                        state_apply(cx.el, cx.kstm, cx.v, S, Sring, B5)
                        if fin_tt is not None:
                            cx.df = pool_fin1(fin_tt)

                    def s9():
                        if fin_tt is not None:
                            pool_fin2(fin_tt, cx.df)

                    return [s0, s1, s2, s3, s4, s5, s6, s7, s8, s9]

                def fin_tile(tt):
                    cx = CX()

                    def s8():
                        cx.df = pool_fin1(tt)

                    def s9():
                        pool_fin2(tt, cx.df)

                    return [None] * 8 + [s8, s9]

                tilesA = []
                if NCTX == 1:
                    raise NotImplementedError
                for i in range(NCTX):
                    tilesA.append(a_tile(ctxs[i * 128:(i + 1) * 128, :], 2, 0, Sf, Sfbf_r, None, None, None))
                for i in reversed(range(NCTX)):
                    tilesA.append(a_tile(ctxs[i * 128:(i + 1) * 128, :], 2, 1, Sb, Sbbf_r, None, None, None))
                for tau in reversed(range(NT_ALL)):
                    tt = tau + HALO
                    tilesA.append(a_tile(xs[tau * 128:(tau + 1) * 128, :], 0, 1, Sb, Sbbf_r,
                                         tau if tau < NT_OWN else None,
                                         tau if tau < NT_OWN + HALO else None,
                                         tt if tt < NT_OWN else None))
                for tt in reversed(range(min(HALO, NT_OWN))):
                    tilesA.append(fin_tile(tt))
                run_pipeline(tilesA, [9, 8, 7, 2, 6, 5, 4, 3, 1, 0])
                with nc.Block() as block:
                    P.emit_phase(block)
            if _STOP == 4:
                return nc

            with ExitStack() as esb:
                def sbb(name, shape, dt):
                    return TB(esb.enter_context(nc.sbuf_tensor("sbb_" + name, list(shape), dt)), name)

                def ringb(name, n, shape, dt):
                    return Ring([sbb("%s%d" % (name, i), shape, dt) for i in range(n)])

                w_out_sb = sbb("w_out_sb", [128, KC, D], BF16)
                w_r_sb = sbb("w_r_sb", [128, KC, 36], BF16)
                dw3 = new_dsem()
                for kc in range(KC):
                    P.dma("pool", w_out_sb.t[:, kc, :], w_out[kc * 128:(kc + 1) * 128, :], dw3, writes=[w_out_sb.b], batch=True)
                P.dma("pool", w_r_sb.t[:], w_r.rearrange("(k p) c -> p k c", p=128), dw3, writes=[w_r_sb.b], batch=True)
                Eq_r = ringb("Eq", 2, [128, 512], F32)
                qb_r = ringb("qb", 3, [128, 2, 2, 2, 128], BF16)
                kb_r = ringb("kb", 2, [128, 4, 128], BF16)
                scm_r = ringb("scm", 2, [128, 8, 128], BF16)
                sg_r = ringb("sg", 6, [128, 512], BF16)
                sl_r = ringb("sl", 2, [128, 512], F32)
                ot_r = ringb("ot", 2, [128, 512], F32)
                mtm_r = ringb("mtm", 3, [128, 512], BF16)
                mixT_r = ringb("mixT", 3, [128, KC, 128], BF16)
                mixT_d = dring(3)
                mt_r = ringb("mt", 2, [128, D], F32)
                x1_r = ringb("x1", 3, [128, D], F32)
                x1_d = dring(3)
                h2_r = ringb("h2T", 3, [128, KC, 128], BF16)
                h2_d = dring(3)
                RG = 4
                assert NT_OWN % RG == 0
                rl_r = ringb("rl", 2, [128, RG, 36], F32)
                rw_r = ringb("rw", 2, [128, RG, 160], F32)
                rstate = {}
                for r_ in qb_r.items:
                    P.op("dve", lambda e, r_=r_: e.memset(r_.t[:], 0.0), writes=[r_.b])

                def b_tile(t):
                    cx = CX()

                    def s0():
                        cx.hT = hT_r.next()
                        P.dma("sp", cx.hT.t[:], hT_scr[t], hT_d[hT_r.i], reads=[hT_b[t]], writes=[cx.hT.b])

                    def s1():
                        hT = cx.hT
                        for c in range(4):
                            proj_fm(hT, c * 128, 128, B1.t[:, c * 128:(c + 1) * 128], B1.b)
                        proj_tm(hT, OFF_V, B2.t[:, :], B2.b)
                        proj_fm(hT, OFF_A, 32, B7a.t[0:32, 0:128], B7a.b)
                        proj_tm(hT, OFF_G, B6.t[:, :], B6.b)
                        cx.qk = qk_r.next()
                        P.cp("act", cx.qk.t[:], B1.t[:, :], reads=[B1.b], writes=[cx.qk.b])
                        cx.v = v_r.next()
                        P.cp("act", cx.v.t[:], B2.t[:, :], reads=[B2.b], writes=[cx.v.b])
                        cx.a = a_r.next()
                        P.cp("act", cx.a.t[0:32, :], B7a.t[0:32, 0:128], reads=[B7a.b], writes=[cx.a.b])
                        cx.sl = sl_r.next()
                        P.act(cx.sl.t[:], B6.t[:, :], AF.Silu, reads=[B6.b], writes=[cx.sl.b])

                    def s2():
                        cx.sg = sg_r.next()
                        P.tt("pool", cx.sg.t[:].rearrange("p (h e) -> p h e", h=4), cx.sl.t[:].rearrange("p (h e) -> p h e", h=4),
                             ggB.t[:, :].unsqueeze(1).to_broadcast([128, 4, 128]), ALU.mult, reads=[cx.sl.b, ggB.b],
                             writes=[cx.sg.b])
                        cx.l, _, _ = decay_mm(cx.a, [0, 1], B7a)

                    def s3():
                        cumsum_mm(cx.l, [0, 1])
                        Ek = cx.Ek = Ek_r.next()
                        Eq = cx.Eq = Eq_r.next()
                        P.act(Ek.t[:, :], B3.t[:, :], AF.Exp, scale=1.0 / 16, reads=[B3.b], writes=[Ek.b])
                        P.act(Eq.t[:, :], B3.t[:, :], AF.Exp, scale=-1.0 / 16, reads=[B3.b], writes=[Eq.b])
                        el = cx.el = el_r.next()
                        cs3 = B3.t[:, :].rearrange("p (j t) -> p j t", t=128)
                        P.act(el.t[:, 0:2], cs3[:, 0:2, 127], AF.Exp, scale=-1.0 / 16, reads=[B3.b], writes=[el.b])
                        cx.sbin = Sbbf_r.next()
                        P.dma("sp", cx.sbin.t[:], S_scr[t], Sbbf_d[Sbbf_r.i], reads=[S_b[t]], writes=[cx.sbin.b])

                    def s4():
                        qk, Ek, Eq, el = cx.qk, cx.Ek, cx.Eq, cx.el
                        kst = cx.kst = kst_r.next()
                        for c in range(2):
                            P.stt("dve", kst.t[:, c, :], qk.t[:, 256 + c * 128:256 + (c + 1) * 128], el.t[:, c:c + 1],
                                  Ek.t[:, c * 128:(c + 1) * 128], ALU.mult, ALU.mult,
                                  reads=[qk.b, el.b, Ek.b], writes=[kst.b])
                        qb = cx.qb = qb_r.next()
                        kb = cx.kb = kb_r.next()
                        for hp in range(2):
                            r0 = hp * 64
                            P.stt("dve", qb.t[r0:r0 + 64, :, :, hp, :],
                                  qk.t[r0:r0 + 64, 0:256].rearrange("p (c t) -> p c t", c=2).unsqueeze(1).to_broadcast([64, 2, 2, 128]),
                                  0.125, Eq.t[r0:r0 + 64, :].rearrange("p (d c t) -> p d c t", d=2, c=2), ALU.mult, ALU.mult,
                                  reads=[qk.b, Eq.b], writes=[qb.b])
                        P.tt("dve", kb.t[:].rearrange("p (d c) t -> p d (c t)", d=2),
                             qk.t[:, 256:512].unsqueeze(1).to_broadcast([128, 2, 256]),
                             Ek.t[:, :].rearrange("p (d x) -> p d x", d=2), ALU.mult, reads=[qk.b, Ek.b], writes=[kb.b])

                    def s5():
                        cx.kstm = kst_transpose(cx.kst, B0)
                        kb, qb = cx.kb, cx.qb
                        scb = (B4, B5)
                        for d in range(2):
                            for h in range(4):
                                c = h // 2
                                P.mm(scb[d].t[:, h * 128:(h + 1) * 128], kb.t[:, d * 2 + c, :],
                                     qb.t[:, d, c, h % 2, :], reads=[kb.b, qb.b], writes=[scb[d].b])
                        scm = cx.scm = scm_r.next()
                        for d in range(2):
                            P.tt("dve", scm.t[:, d * 4:(d + 1) * 4, :], scb[d].t[:, :].rearrange("p (h t) -> p h t", h=4),
                                 tri[d].t[:, :].unsqueeze(1).to_broadcast([128, 4, 128]), ALU.mult,
                                 reads=[scb[d].b, tri[d].b], writes=[scm.b])

                    def s6():
                        scm, v, qb, sbin = cx.scm, cx.v, cx.qb, cx.sbin
                        sfbf = Sfbf_r.cur()
                        for h in range(4):
                            c = h // 2
                            oo = B6.t[:, h * 128:(h + 1) * 128]
                            P.mm(oo, scm.t[:, h, :], v.t[:, h * 128:(h + 1) * 128], start=True, stop=False,
                                 reads=[scm.b, v.b], writes=[B6.b])
                            P.mm(oo, scm.t[:, 4 + h, :], v.t[:, h * 128:(h + 1) * 128], start=False, stop=False,
                                 reads=[scm.b, v.b], writes=[B6.b])
                            P.mm(oo, qb.t[:, 1, c, h % 2, :], sbin.t[:, c, :], start=False, stop=False,
                                 reads=[qb.b, sbin.b], writes=[B6.b])
                            P.mm(oo, qb.t[:, 0, c, h % 2, :], sfbf.t[:, c, :], start=False, stop=True,
                                 reads=[qb.b, sfbf.b], writes=[B6.b])
                        st = stat_r.next()
                        for h in range(4):
                            P.act(sqj.t[:, h * 128:(h + 1) * 128], B6.t[:, h * 128:(h + 1) * 128], AF.Square,
                                  accum_out=st.t[:, h:h + 1], reads=[B6.b], writes=[st.b])
                        P.act(st.t[:, 0:4], st.t[:, 0:4], AF.Ln, bias=EPS, scale=1.0 / 128, reads=[st.b], writes=[st.b])
                        P.act(st.t[:, 0:4], st.t[:, 0:4], AF.Exp, scale=-0.5, reads=[st.b], writes=[st.b])
                        ot = cx.ot = ot_r.next()
                        P.tt("dve", ot.t[:].rearrange("p (h e) -> p h e", h=4), B6.t[:, :].rearrange("p (h e) -> p h e", h=4),
                             st.t[:, 0:4].unsqueeze(2).to_broadcast([128, 4, 128]), ALU.mult, reads=[B6.b, st.b], writes=[ot.b])

                    def s7():
                        state_apply(cx.el, cx.kstm, cx.v, Sf, Sfbf_r, B3)
                        cx.mtm = mtm_r.next()
                        P.tt("pool", cx.mtm.t[:], cx.ot.t[:], cx.sg.t[:], ALU.mult, reads=[cx.ot.b, cx.sg.b], writes=[cx.mtm.b])
                        cx.mixT = mixT_r.next()
                        P.dma("sp", cx.mixT.t[:, 4:8, :], pl_scr[t], mixT_d[mixT_r.i], reads=[pl_b[t]], writes=[cx.mixT.b])

                    def s8():
                        mtm, mixT = cx.mtm, cx.mixT
                        for h in range(4):
                            P.tr(B0.t[:, h * 128:(h + 1) * 128], mtm.t[:, h * 128:(h + 1) * 128], ident.t[:],
                                 reads=[mtm.b, ident.b], writes=[B0.b])
                        P.cp("act", mixT.t[:, 0:4, :].rearrange("p k t -> p (k t)"), B0.t[:, 0:512], reads=[B0.b],
                             writes=[mixT.b])
                        cx.xt = load_x(xs[t * 128:(t + 1) * 128, :])

                    def s9():
                        mixT, xt = cx.mixT, cx.xt
                        ob = (B1, B2)
                        for half in range(2):
                            for kc in range(KC):
                                P.mm(ob[half].t[:, :], mixT.t[:, kc, :], w_out_sb.t[:, kc, half * 512:(half + 1) * 512],
                                     start=(kc == 0), stop=(kc == KC - 1), reads=[mixT.b, w_out_sb.b], writes=[ob[half].b])
                        x1 = cx.x1 = x1_r.next()
                        mt = mt_r.next()
                        for half in range(2):
                            P.tt("dve", mt.t[:, half * 512:(half + 1) * 512], ob[half].t[:, :],
                                 gtB.t[:, 0, half * 512:(half + 1) * 512], ALU.mult, reads=[ob[half].b, gtB.b], writes=[mt.b])
                        P.tt("pool", x1.t[:], mt.t[:], xt.t[:], ALU.add, reads=[mt.b, xt.b], writes=[x1.b])
                        P.dma("pool", y[t * 128:(t + 1) * 128, :], x1.t[:], x1_d[x1_r.i], reads=[x1.b], writes=[y_b[t]])

                    def s10():
                        cx.st = norm_act(cx.x1)

                    def s10b():
                        cx.xn = norm_dve(cx.x1, cx.st)

                    def s11():
                        cx.h2 = h2_r.next()
                        transpose_mod(cx.xn, 4, cx.h2)
                        P.dma("pool", h2_scr[:, :, t * 128:(t + 1) * 128], cx.h2.t[:], h2_d[h2_r.i], reads=[cx.h2.b],
                              writes=[h2_b[t]])

                    def s12():
                        h2 = cx.h2
                        for kc in range(KC):
                            P.mm(B7a.t[:, 128:164], h2.t[:, kc, :], w_r_sb.t[:, kc, :], start=(kc == 0), stop=(kc == KC - 1),
                                 reads=[h2.b, w_r_sb.b], writes=[B7a.b])
                        if t % RG == 0:
                            rstate["L"] = rl_r.next()
                        L = rstate["L"]
                        cx.L = L
                        P.cp("dve", L.t[:, t % RG, :], B7a.t[:, 128:164], reads=[B7a.b], writes=[L.b])

                    def r13():
                        L = cx.L
                        Wt = cx.W = rw_r.next()
                        W = Wt.t
                        rb = [Wt.b]
                        lg = L.t[:, :, 0:4]
                        gmax_b = W[:, :, 4:5].to_broadcast([128, RG, 4])
                        P.op("dve", lambda e: e.tensor_reduce(W[:, :, 4:5], lg, AX.X, ALU.max), reads=[L.b], writes=rb)
                        P.tt("dve", W[:, :, 8:12], lg, gmax_b, ALU.is_equal, reads=[L.b] + rb, writes=rb)
                        P.tt("dve", W[:, :, 12:16], lg, gmax_b, ALU.subtract, reads=[L.b] + rb, writes=rb)
                        P.act(W[:, :, 16:20], W[:, :, 12:16], AF.Exp, reads=rb, writes=rb)

                    def r14():
                        L = cx.L
                        W = cx.W.t
                        rb = [cx.W.b]
                        le4 = L.t[:, :, 4:36].rearrange("p g (a j) -> p g a j", a=4)
                        P.op("dve", lambda e: e.tensor_reduce(W[:, :, 5:6], W[:, :, 16:20], AX.X, ALU.add), reads=rb, writes=rb)
                        P.op("dve", lambda e: e.reciprocal(W[:, :, 6:7], W[:, :, 5:6]), reads=rb, writes=rb)
                        P.ts("dve", W[:, :, 20:24], W[:, :, 8:12], 1.0, BIG, ALU.subtract, ALU.mult, reads=rb, writes=rb)
                        P.tt("dve", W[:, :, 32:64].rearrange("p g (a j) -> p g a j", a=4), le4,
                             W[:, :, 20:24].unsqueeze(3).to_broadcast([128, RG, 4, 8]), ALU.add, reads=[L.b] + rb, writes=rb)
                        P.op("dve", lambda e: e.tensor_reduce(W[:, :, 24:25], W[:, :, 32:64], AX.X, ALU.max), reads=rb, writes=rb)
                        P.tt("dve", W[:, :, 64:96], W[:, :, 32:64], W[:, :, 24:25].to_broadcast([128, RG, 32]), ALU.is_equal,
                             reads=rb, writes=rb)
                        P.stt("dve", W[:, :, 96:128], W[:, :, 64:96], -BIG, W[:, :, 32:64], ALU.mult, ALU.add, reads=rb, writes=rb)
                        P.op("dve", lambda e: e.tensor_reduce(W[:, :, 25:26], W[:, :, 96:128], AX.X, ALU.max), reads=rb, writes=rb)
                        P.tt("dve", W[:, :, 128:160], W[:, :, 96:128], W[:, :, 25:26].to_broadcast([128, RG, 32]), ALU.is_equal,
                             reads=rb, writes=rb)
                        P.tt("dve", W[:, :, 26:27], W[:, :, 25:26], W[:, :, 24:25], ALU.subtract, reads=rb, writes=rb)
                        P.act(W[:, :, 27:28], W[:, :, 26:27], AF.Exp, reads=rb, writes=rb)

                    def r15():
                        W = cx.W.t
                        rb = [cx.W.b]
                        P.ts("dve", W[:, :, 0:1], W[:, :, 27:28], 1.0, None, ALU.add, reads=rb, writes=rb)
                        P.op("dve", lambda e: e.reciprocal(W[:, :, 1:2], W[:, :, 0:1]), reads=rb, writes=rb)
                        P.tt("dve", W[:, :, 2:3], W[:, :, 1:2], W[:, :, 6:7], ALU.mult, reads=rb, writes=rb)
                        P.tt("dve", W[:, :, 3:4], W[:, :, 2:3], W[:, :, 27:28], ALU.mult, reads=rb, writes=rb)
                        P.tt("dve", W[:, :, 32:64], W[:, :, 64:96], W[:, :, 2:3].to_broadcast([128, RG, 32]), ALU.mult,
                             reads=rb, writes=rb)
                        P.tt("dve", W[:, :, 96:128], W[:, :, 128:160], W[:, :, 3:4].to_broadcast([128, RG, 32]), ALU.mult,
                             reads=rb, writes=rb)
                        P.tt("dve", gates.t[:, t - RG + 1:t + 1, :], W[:, :, 32:64], W[:, :, 96:128], ALU.add,
                             reads=rb, writes=[gates.b])

                    if t % RG != RG - 1:
                        r13 = r14 = r15 = None
                    return [s0, s1, s2, s3, s4, s5, s6, s7, s8, s9, s10, s10b, s11, s12, r13, r14, r15]

                tilesB = [b_tile(t) for t in range(NT_OWN)]
                run_pipeline(tilesB, [7, 12, 11, 10, 9, 8, 6, 5, 4, 3, 2, 1, 0, 16, 15, 14, 13])
                with nc.Block() as block:
                    P.emit_phase(block)
        if _STOP == 2:
            return nc
        wes.close()

        with ExitStack() as es:
            def sb(name, shape, dt):
                return TB(es.enter_context(nc.sbuf_tensor("sb_" + name, list(shape), dt)), name)

            def ps(name, shape, dt=F32):
                return TB(es.enter_context(nc.psum_tensor("ps_" + name, list(shape), dt)), name)

            NSUB = TPB // 4 if TPB >= 4 else 1
            TPS = TPB // NSUB
            h2sub = [sb("h2blk%d" % i, [128, KC, TPS * 128], BF16) for i in range(NSUB)]
            dh2 = [new_dsem() for _ in range(NSUB)]
            yacc = [sb("yacc%d" % i, [128, D], F32) for i in range(TPB)]
            wi_r = Ring([sb("wi%d" % i, [128, KC, 2 * DEXP], BF16) for i in range(2)])
            wo_r = Ring([sb("wo%d" % i, [128, 2, D], BF16) for i in range(2)])
            we_d = [new_dsem(), new_dsem()]
            sa_r = Ring([sb("sa%d" % i, [128, 512], F32) for i in range(2)])
            gT_r = Ring([sb("gT%d" % i, [128, 2, 512], BF16) for i in range(3)])
            x1t_r = Ring([sb("x1t%d" % i, [128, D], F32) for i in range(5)])
            x1t_d = [new_dsem() for _ in range(5)]
            sqj2 = sb("sqj2", [128, D], BF16)
            st2_r = Ring([sb("st2%d" % i, [128, 4], F32) for i in range(4)])
            yo_r = Ring([sb("yo%d" % i, [128, D], F32) for i in range(3)])
            yo_d = [new_dsem() for _ in range(3)]
            pa = [ps("pa%d" % q, [128, 512]) for q in range(2)]
            pu = [ps("pu%d" % q, [128, 512]) for q in range(2)]
            po_r = Ring([(ps("po%da" % i, [128, 512]), ps("po%db" % i, [128, 512])) for i in range(2)])
            out_dsems = yo_d

            def load_h2(blk, sub):
                tok0 = (blk * TPB + sub * TPS) * 128
                P.dma("sp", h2sub[sub].t[:], h2_scr[:, :, tok0:tok0 + TPS * 128], dh2[sub],
                      reads=[h2_b[blk * TPB + sub * TPS + i] for i in range(TPS)], writes=[h2sub[sub].b])

            fin_tiles = []

            NFS = 4

            def fin_stage(ft):
                stage, blk, ti, cx = ft
                tg = blk * TPB + ti
                if stage == 0:
                    x1t = cx["x1t"] = x1t_r.next()
                    P.dma("sp", x1t.t[:], y[tg * 128:(tg + 1) * 128, :], x1t_d[x1t_r.i], reads=[y_b[tg]], writes=[x1t.b])
                elif stage == 1:
                    x1t = cx["x1t"]
                    P.tt("pool", x1t.t[:], x1t.t[:], yacc[ti].t[:], ALU.add, reads=[x1t.b, yacc[ti].b], writes=[x1t.b])
                elif stage == 2:
                    x1t = cx["x1t"]
                    st = cx["st"] = st2_r.next()
                    P.act(sqj2.t[:], x1t.t[:], AF.Square, accum_out=st.t[:, 0:1], reads=[x1t.b], writes=[st.b])
                    P.act(st.t[:, 1:2], st.t[:, 0:1], AF.Ln, bias=EPS, scale=1.0 / D, reads=[st.b], writes=[st.b])
                    P.act(st.t[:, 2:3], st.t[:, 1:2], AF.Exp, scale=-0.5, reads=[st.b], writes=[st.b])
                else:
                    x1t, st = cx["x1t"], cx["st"]
                    yo = yo_r.next()
                    P.stt("dve", yo.t[:], x1t.t[:], st.t[:, 2:3], fgB.t[:], ALU.mult, ALU.mult,
                          reads=[x1t.b, st.b, fgB.b], writes=[yo.b])
                    P.dma("sp", y[tg * 128:(tg + 1) * 128, :], yo.t[:], yo_d[yo_r.i], reads=[yo.b], writes=[y_b[tg]])
                ft[0] = stage + 1

            def fin_step():
                todo = [ft for ft in fin_tiles if ft[0] < NFS][:NFS]
                for ft in todo:
                    fin_stage(ft)
                return len(todo) > 0

            def fin_require(blk, ti):
                for ft in fin_tiles:
                    if ft[1] == blk and ft[2] == ti:
                        while ft[0] < 2:
                            fin_step()

            def moe_down(blk, e_, sub, gT, wo, part=None):
                hs = max(1, TPS // 2)
                jr = range(TPS) if part is None else (range(0, hs) if part == 0 else range(hs, TPS))
                last_part = part is None or part == 1
                for j in jr:
                    ti = sub * TPS + j
                    tg = blk * TPB + ti
                    po = po_r.next()
                    for half in range(2):
                        for q in range(2):
                            P.mm(po[half].t[:, :], gT.t[:, q, j * 128:(j + 1) * 128],
                                 wo.t[:, q, half * 512:(half + 1) * 512], start=(q == 0), stop=(q == 1),
                                 reads=[gT.b, wo.b], writes=[po[half].b])
                    if e_ == 0 and blk > 0:
                        fin_require(blk - 1, ti)
                    for half in range(2):
                        ya = yacc[ti].t[:, half * 512:(half + 1) * 512]
                        if e_ == 0:
                            P.ts("dve", ya, po[half].t[:, :], gates.t[:, tg, e_:e_ + 1], None, ALU.mult,
                                 reads=[po[half].b, gates.b], writes=[yacc[ti].b])
                        else:
                            P.stt("dve", ya, po[half].t[:, :], gates.t[:, tg, e_:e_ + 1], ya, ALU.mult, ALU.add,
                                  reads=[po[half].b, gates.b, yacc[ti].b], writes=[yacc[ti].b])
                if not last_part:
                    return
                if e_ == NEXP - 1:
                    for j in range(TPS):
                        fin_tiles.append([0, blk, sub * TPS + j, {}])
                for _ in range(4):
                    fin_step()

            pending = None
            for sub in range(NSUB):
                load_h2(0, sub)
            seq = [(b_, e_) for b_ in range(NBLK) for e_ in range(NEXP)]

            def issue_w(idx):
                e_ = seq[idx][1]
                wi = wi_r.next()
                wo = wo_r.next()
                P.dma("pool", wi.t[:], w_ei[e_].rearrange("(k p) c -> p k c", p=128), we_d[wi_r.i], writes=[wi.b])
                P.dma("pool", wo.t[:], w_eo[e_].rearrange("(k p) c -> p k c", p=128), we_d[wi_r.i], writes=[wo.b],
                      batch=True)
                P.tt("pool", wo.t[:], wo.t[:], gtB.t[:, 1, :].unsqueeze(1).to_broadcast([128, 2, D]), ALU.mult,
                     reads=[wo.b, gtB.b], writes=[wo.b])
                return wi, wo

            wcur = issue_w(0)
            if True:
                for idx, (blk, e_) in enumerate(seq):
                    wi, wo = wcur
                    for sub in range(NSUB):
                        ns = TPS * 128
                        hb = h2sub[sub]
                        gT = gT_r.next()
                        for q in range(2):
                            for kc in range(KC):
                                P.mm(pa[q].t[:, 0:ns], wi.t[:, kc, q * 128:(q + 1) * 128], hb.t[:, kc, :],
                                     start=(kc == 0), stop=(kc == KC - 1), reads=[wi.b, hb.b], writes=[pa[q].b])
                            for kc in range(KC):
                                P.mm(pu[q].t[:, 0:ns], wi.t[:, kc, DEXP + q * 128:DEXP + (q + 1) * 128],
                                     hb.t[:, kc, :], start=(kc == 0), stop=(kc == KC - 1),
                                     reads=[wi.b, hb.b], writes=[pu[q].b])
                            sa = sa_r.next()
                            P.act(sa.t[:, 0:ns], pa[q].t[:, 0:ns], AF.Silu, reads=[pa[q].b], writes=[sa.b])
                            P.tt("dve", gT.t[:, q, 0:ns], sa.t[:, 0:ns], pu[q].t[:, 0:ns], ALU.mult,
                                 reads=[sa.b, pu[q].b], writes=[gT.b])
                            if pending is not None:
                                moe_down(*pending, part=q)
                        if e_ == NEXP - 1 and blk + 1 < NBLK:
                            load_h2(blk + 1, sub)
                        pending = (blk, e_, sub, gT, wo)
                        if sub == 0 and idx + 1 < len(seq):
                            wcur = issue_w(idx + 1)
            if pending is not None:
                moe_down(*pending)
                pending = None
            while fin_step():
                pass
            with nc.Block() as block:
                P.emit_phase(block, final=out_dsems)
    return nc


def _win_matrix(n, w):
    lo = w // 2
    hi = w - 1 - lo
    M = np.zeros((n, n), np.float32)
    for i in range(n):
        s = min(max(i - lo, 0), n)
        e = min(max(i + hi + 1, 0), n)
        M[i, s:e] = 1.0 / float(e - s)
    return M


def _pool_blocks(rows_total, par):
    blocks = np.zeros((NBLK_A, 128, 128), np.float32)
    eye = np.eye(128, dtype=np.float32)
    for gi, (w, R) in enumerate(zip(POOL_W, POOL_R)):
        Mr = _win_matrix(rows_total, w)
        Mc = _win_matrix(GRID_W, w)
        if par:
            Mr = Mr[::-1, ::-1]
            Mc = Mc[::-1, ::-1]
        Acol = np.ascontiguousarray(Mc.T)
        for v in range(R + 1):
            t = v
            for dt in range(-R, R + 1):
                if (v, gi, dt) not in POOL_TAB:
                    continue
                blk = np.zeros((2, GRID_W, 2, GRID_W), np.float32)
                for a in range(2):
                    r_in = 2 * (t + dt) + a
                    for b2 in range(2):
                        r_out = 2 * t + b2
                        if r_in < rows_total and r_out < rows_total:
                            blk[a, :, b2, :] = Mr[r_out, r_in] * Acol
                blk = blk.reshape(128, 128)
                if dt == 0:
                    blk = blk - eye
                blocks[POOL_TAB[(v, gi, dt)]] = blk
    return blocks


_PROG_CACHE = {}


def kernel(x, c, ctx, c_ctx, w_ada, b_ada, norm1_g, w_in, w_decay, b_decay, gla_norm_g, w_pool, pool_scale,
           w_out, norm2_g, w_router_group, w_router_expert, w_expert_in, w_expert_out, final_norm_g):
    f = lambda a: np.ascontiguousarray(np.asarray(a, dtype=np.float32))
    x, c, ctx, c_ctx = f(x), f(c), f(ctx), f(c_ctx)
    Bsz, SEQ, _ = x.shape
    NT = SEQ // 128
    NT_OWN = NT // 2
    NT_OTH = NT - NT_OWN
    NCTX = ctx.shape[1] // 128
    rows_total = SEQ // GRID_W
    n_cores = 2 * Bsz
    key = (NT_OWN, NT_OTH, NCTX)
    if key not in _PROG_CACHE:
        _PROG_CACHE[key] = build_program(*key)
    nc = _PROG_CACHE[key]

    w_ada0, b_ada0 = f(w_ada)[0], f(b_ada)[0]
    w_in0 = f(w_in)[0]
    w_dec0, b_dec0 = f(w_decay)[0], f(b_decay)[0]
    tri_f = np.triu(np.ones((128, 128), np.float32))
    cmat = np.stack([np.eye(128, dtype=np.float32), tri_f, np.ascontiguousarray(tri_f.T)])
    common = {
        "w_ada": w_ada0,
        "b_adaT": np.ascontiguousarray(b_ada0.reshape(48, 128).T),
        "b_gtB": np.ascontiguousarray(np.broadcast_to(
            np.stack([b_ada0[2 * D:3 * D], b_ada0[5 * D:6 * D]])[None], (128, 2, D))),
        "g12T": np.ascontiguousarray(np.stack([f(norm1_g)[0].reshape(KC, 128).T, f(norm2_g)[0].reshape(KC, 128).T], axis=1)),
        "ggB": np.ascontiguousarray(np.broadcast_to(f(gla_norm_g)[0][None, :], (128, 128))),
        "w_pool": f(w_pool)[0],
        "pscT": np.ascontiguousarray(f(pool_scale)[0].reshape(4, 128).T),
        "w_out": f(w_out)[0],
        "w_r": np.ascontiguousarray(np.concatenate([f(w_router_group)[0], f(w_router_expert)[0]], axis=1)),
        "w_ei": f(w_expert_in)[0],
        "w_eo": f(w_expert_out)[0],
        "fgB": np.ascontiguousarray(np.broadcast_to(f(final_norm_g)[None, :], (128, D))),
        "cmat": cmat,
    }
    per_par = []
    for par in range(2):
        d0, d1 = (0, 1) if par == 0 else (1, 0)
        wi = w_in0.copy()
        if par:
            wi[:, OFF_A:OFF_A + 16] = w_in0[:, OFF_A + 16:OFF_A + 32]
            wi[:, OFF_A + 16:OFF_A + 32] = w_in0[:, OFF_A:OFF_A + 16]
        wdec = np.zeros((33, 512), np.float32)
        wdec[0:16, 0:256] = w_dec0[d0]
        wdec[16:32, 256:512] = w_dec0[d1]
        wdec[32, 0:256] = b_dec0[d0]
        wdec[32, 256:512] = b_dec0[d1]
        per_par.append({"w_in": wi, "wdec": wdec, "apool": _pool_blocks(rows_total, par)})
    in_maps = []
    for core in range(n_cores):
        b, par = core // 2, core % 2
        m = dict(common)
        m.update(per_par[par])
        xb, cb = x[b], ctx[b]
        if par:
            xb, cb = xb[::-1], cb[::-1]
        m["xs"] = np.ascontiguousarray(xb)
        m["ctxs"] = np.ascontiguousarray(cb)
        m["cvec"] = np.ascontiguousarray(np.stack([c[b].reshape(KC, 128).T, c_ctx.reshape(KC, 128).T], axis=2))
        in_maps.append(m)
    res = run_bass_kernel_spmd(nc, in_maps, core_ids=list(range(n_cores)))
    out = np.empty((Bsz, SEQ, D), np.float32)
    half = NT_OWN * 128
    for core in range(n_cores):
        b, par = core // 2, core % 2
        yv = res.results[core]["y"]
        if par == 0:
            out[b, :half] = yv
        else:
            out[b, half:] = yv[::-1]
    return out
```

```python
import numpy as np
import concourse.bass as bass
import concourse.mybir as mybir
from concourse.bass_utils import run_bass_kernel_spmd

F32 = mybir.dt.float32
BF16 = mybir.dt.bfloat16
AF = mybir.ActivationFunctionType
ALU = mybir.AluOpType
AX = mybir.AxisListType


class Buf:
    __slots__ = ("name", "last_w", "readers")

    def __init__(self, name):
        self.name = name
        self.last_w = None
        self.readers = []


class DSem:
    __slots__ = ("sem", "count", "open_ops")

    def __init__(self, sem):
        self.sem = sem
        self.count = 0
        self.open_ops = []


class Op:
    __slots__ = ("eng", "fn", "deps", "signal", "semval", "dsem", "is_dma", "idx")


class Prog:
    ENGS = ("pe", "act", "dve", "pool", "sp")

    def __init__(self, nc):
        self.nc = nc
        self.ops = []
        self.by_eng = {e: [] for e in self.ENGS}

    def op(self, eng, fn, reads=(), writes=(), dsem=None, batch=False):
        o = Op()
        o.eng = eng
        o.fn = fn
        o.deps = []
        o.signal = False
        o.semval = None
        o.dsem = dsem
        o.is_dma = dsem is not None
        o.idx = len(self.ops)
        if o.is_dma:
            dsem.count += 16
            o.semval = dsem.count
            o.signal = True
            if batch:
                for q in dsem.open_ops:
                    q.semval = dsem.count
            else:
                dsem.open_ops = []
            dsem.open_ops.append(o)
        deps = set()
        for b in reads:
            if b.last_w is not None:
                deps.add(b.last_w)
        for b in writes:
            if b.last_w is not None:
                deps.add(b.last_w)
            for r in b.readers:
                deps.add(r)
        same_batch = set(id(q) for q in dsem.open_ops) if (o.is_dma and batch) else ()
        for p in deps:
            if p is o or id(p) in same_batch:
                continue
            if p.idx < getattr(self, "n_emitted_ops", 0):
                continue
            if (not p.is_dma) and (not o.is_dma) and p.eng == eng:
                if eng == "pe":
                    continue
                raw = any(b.last_w is p for b in reads) or any(b.last_w is p for b in writes)
                if not raw:
                    continue
            o.deps.append(p)
            p.signal = True
        for b in reads:
            b.readers.append(o)
        for b in writes:
            b.last_w = o
            b.readers = []
        self.ops.append(o)
        self.by_eng[eng].append(o)
        return o

    def emit(self, block, sems, final=()):
        self.sems = sems
        self.bar = None
        self.emit_phase(block, final=final)

    def emit_phase(self, block, final=()):
        sems = self.sems
        if not hasattr(self, "cnt"):
            self.cnt = {e: 0 for e in self.ENGS}
            self.waited = {e: {} for e in self.ENGS}
            self.emitted = {e: 0 for e in self.ENGS}
            self.phase = 0
        self.phase += 1
        self.n_emitted_ops = len(self.ops)
        streams = {}
        for e in self.ENGS:
            streams[e] = self.by_eng[e][self.emitted[e]:]
            self.emitted[e] = len(self.by_eng[e])
            c = self.cnt[e]
            for o in streams[e]:
                if (not o.is_dma) and o.signal:
                    c += 1
                    o.semval = c
            self.cnt[e] = c
        prog = self
        phase = self.phase

        def run(engname, engine):
            waited = prog.waited[engname]
            my_dsems = {}
            for o in streams[engname]:
                for p in o.deps:
                    if p.is_dma:
                        s, v = p.dsem.sem, p.semval
                    else:
                        s, v = sems[p.eng], p.semval
                    key = s.num
                    if waited.get(key, 0) >= v:
                        continue
                    waited[key] = v
                    engine.wait_ge(s, v)
                ins = o.fn(engine)
                if o.is_dma:
                    ins.then_inc(o.dsem.sem, 16)
                    my_dsems[o.dsem.sem.num] = (o.dsem.sem, o.semval)
                elif o.signal:
                    ins.then_inc(sems[engname], 1)
            for s, v in my_dsems.values():
                if waited.get(s.num, 0) < v:
                    waited[s.num] = v
                    engine.wait_ge(s, v)
            if engname == "sp":
                for d in final:
                    engine.wait_ge(d.sem, d.count)
            if prog.bar is not None:
                engine.sem_inc(prog.bar, 1)
                engine.wait_ge(prog.bar, 5 * phase)

        @block.tensor
        def _(eng):
            run("pe", eng)

        @block.scalar
        def _(eng):
            run("act", eng)

        @block.vector
        def _(eng):
            run("dve", eng)

        @block.gpsimd
        def _(eng):
            run("pool", eng)

        @block.sync
        def _(eng):
            run("sp", eng)

    def mm(self, out, lhsT, rhs, start=True, stop=True, reads=(), writes=()):
        return self.op("pe", lambda e: e.matmul(out, lhsT, rhs, start=start, stop=stop), reads, writes)

    def tr(self, out, in_, ident, reads=(), writes=()):
        return self.op("pe", lambda e: e.transpose(out, in_, ident), reads, writes)

    def act(self, out, in_, func, reads=(), writes=(), **kw):
        return self.op("act", lambda e: e.activation(out, in_, func, **kw), reads, writes)

    def tt(self, eng, out, in0, in1, op, reads=(), writes=()):
        return self.op(eng, lambda e: e.tensor_tensor(out, in0, in1, op), reads, writes)

    def ts(self, eng, out, in0, s1, s2, op0, op1=None, reads=(), writes=()):
        if op1 is None:
            return self.op(eng, lambda e: e.tensor_scalar(out, in0, s1, s2, op0), reads, writes)
        return self.op(eng, lambda e: e.tensor_scalar(out, in0, s1, s2, op0, op1), reads, writes)

    def stt(self, eng, out, in0, scalar, in1, op0, op1, reads=(), writes=()):
        return self.op(eng, lambda e: e.scalar_tensor_tensor(out, in0, scalar, in1, op0, op1), reads, writes)

    def cp(self, eng, out, in_, reads=(), writes=()):
        if eng == "act":
            return self.op(eng, lambda e: e.copy(out, in_), reads, writes)
        return self.op(eng, lambda e: e.tensor_copy(out, in_), reads, writes)

    def dma(self, eng, out, in_, dsem, reads=(), writes=(), batch=False):
        return self.op(eng, lambda e: e.dma_start(out=out, in_=in_), reads, writes, dsem=dsem, batch=batch)

    def reset_tracking(self, bufs):
        for b in bufs:
            b.last_w = None
            b.readers = []


class TB:
    __slots__ = ("t", "b")

    def __init__(self, t, name):
        self.t = t
        self.b = Buf(name)


class Ring:
    def __init__(self, items):
        self.items = items
        self.i = -1

    def next(self):
        self.i = (self.i + 1) % len(self.items)
        return self.items[self.i]

    def cur(self):
        return self.items[self.i]


D = 1024
KC = 8
GRID_W = 64
IN_COLS = 2080
OFF_Q, OFF_K, OFF_V, OFF_G, OFF_A, OFF_P = 0, 256, 512, 1024, 1536, 1568
NEXP = 32
DEXP = 256
EPS = 1e-6
POOL_W = (2, 4, 8, 16)
POOL_R = (1, 1, 2, 4)
HALO = 4
BIG = 1.0e30


def pool_block_table():
    tab = {}
    n = 0
    for gi, R in enumerate(POOL_R):
        for v in range(R + 1):
            for dt in range(-R, R + 1):
                if v < R and v + dt < 0:
                    continue
                tab[(v, gi, dt)] = n
                n += 1
    return tab, n


POOL_TAB, NBLK_A = pool_block_table()


_STOP = 0


def build_program(NT_OWN, NT_OTH, NCTX):
    from contextlib import ExitStack
    NT_ALL = NT_OWN + NT_OTH
    NTOK = NT_OWN * 128
    TPB = min(16, NT_OWN)
    NBLK = NT_OWN // TPB
    nc = bass.Bass("TRN2", target_bir_lowering=False)

    def din(name, shape, dt=F32):
        return nc.dram_tensor(name, list(shape), dt, kind="ExternalInput").ap()

    xs = din("xs", [NT_ALL * 128, D])
    ctxs = din("ctxs", [NCTX * 128, D])
    cvec = din("cvec", [128, KC, 2])
    w_ada = din("w_ada", [D, 6 * D])
    b_adaT = din("b_adaT", [128, 48])
    b_gtB = din("b_gtB", [128, 2, D])
    g12T = din("g12T", [128, 2, KC])
    w_in = din("w_in", [D, IN_COLS])
    wdec = din("wdec", [33, 512])
    ggB_d = din("ggB", [128, 128])
    w_pool = din("w_pool", [4, 128, 128])
    pscT = din("pscT", [128, 4])
    w_out = din("w_out", [D, D])
    w_r = din("w_r", [D, 36])
    w_ei = din("w_ei", [NEXP, D, 2 * DEXP])
    w_eo = din("w_eo", [NEXP, DEXP, D])
    fgB_d = din("fgB", [128, D])
    cmat = din("cmat", [3, 128, 128])
    apool = din("apool", [NBLK_A, 128, 128])
    y = nc.dram_tensor("y", [NTOK, D], F32, kind="ExternalOutput").ap()
    hT_scr = nc.dram_tensor("hT_scr", [NT_OWN, 128, KC, 128], BF16).ap()
    pl_scr = nc.dram_tensor("pl_scr", [NT_OWN, 128, 4, 128], BF16).ap()
    S_scr = nc.dram_tensor("S_scr", [NT_OWN, 128, 2, 128], BF16).ap()
    h2_scr = nc.dram_tensor("h2_scr", [128, KC, NTOK], BF16).ap()

    ges = ExitStack()
    with ges:
        def gsb(name, shape, dt):
            return TB(ges.enter_context(nc.sbuf_tensor("sg_" + name, list(shape), dt)), name)

        def gsem(name):
            return ges.enter_context(nc.semaphore(name))

        P = Prog(nc)
        P.sems = {e: gsem("s_" + e) for e in Prog.ENGS}
        P.bar = gsem("bar")
        dsem_pool = [DSem(gsem("d%d" % i)) for i in range(70)]
        dsem_i = [0]

        def new_dsem():
            d = dsem_pool[dsem_i[0]]
            dsem_i[0] += 1
            return d

        hT_b = [Buf("hTs%d" % i) for i in range(NT_OWN)]
        pl_b = [Buf("pls%d" % i) for i in range(NT_OWN)]
        S_b = [Buf("Ss%d" % i) for i in range(NT_OWN)]
        h2_b = [Buf("h2s%d" % i) for i in range(NT_OWN)]
        y_b = [Buf("ys%d" % i) for i in range(NT_OWN)]

        ident = gsb("ident", [128, 128], BF16)
        triF = gsb("triF", [128, 128], BF16)
        triB = gsb("triB", [128, 128], BF16)
        tri = (triF, triB)
        modv = gsb("modv", [128, 6, KC], F32)
        gtB = gsb("gtB", [128, 2, D], F32)
        fgB = gsb("fgB", [128, D], F32)
        ggB = gsb("ggB", [128, 128], F32)
        psc = gsb("psc", [128, 4], F32)
        gates = gsb("gates", [128, NT_OWN, NEXP], F32)
        dconst = new_dsem()
        dconst_p = new_dsem()
        for tbv, src in ((ident, cmat[0]), (triF, cmat[1]), (triB, cmat[2])):
            P.dma("pool", tbv.t[:], src, dconst_p, writes=[tbv.b], batch=True)
        P.dma("sp", fgB.t[:], fgB_d, dconst, writes=[fgB.b], batch=True)
        P.dma("sp", ggB.t[:], ggB_d, dconst, writes=[ggB.b], batch=True)
        P.dma("sp", psc.t[:], pscT, dconst, writes=[psc.b], batch=True)

        wes = ExitStack()
        w_in_sb = TB(wes.enter_context(nc.sbuf_tensor("sg_w_in_sb", [128, KC, IN_COLS], BF16)), "w_in_sb")
        wdec_sb = TB(wes.enter_context(nc.sbuf_tensor("sg_wdec_sb", [33, 512], BF16)), "wdec_sb")
        dw = new_dsem()
        for kc in range(KC):
            P.dma("pool", w_in_sb.t[:, kc, :], w_in[kc * 128:(kc + 1) * 128, :], dw, writes=[w_in_sb.b], batch=True)
        P.dma("pool", wdec_sb.t[:], wdec, dw, writes=[wdec_sb.b], batch=True)

        with ExitStack() as es:
            def sb(name, shape, dt):
                return TB(es.enter_context(nc.sbuf_tensor("sb_" + name, list(shape), dt)), name)

            def ps(name, shape, dt=F32):
                return TB(es.enter_context(nc.psum_tensor("ps_" + name, list(shape), dt)), name)

            cv = sb("cv", [128, KC, 2], F32)
            sv = sb("sv", [128, KC, 2], F32)
            srep = sb("srep", [128, KC, 128], F32)
            wst = Ring([sb("wst%d" % i, [128, KC, 512], F32) for i in range(3)])
            wst_d = [new_dsem(), new_dsem(), new_dsem()]
            wst_q = ("sp", "act", "sp")
            bT = sb("bT", [128, 48], F32)
            bgt = sb("bgt", [128, 2, D], F32)
            g12 = sb("g12", [128, 2, KC], F32)
            mv = sb("mv", [128, 4, KC, 2], F32)
            psg = Ring([ps("psg%d" % i, [128, 512]) for i in range(2)])
            psv = Ring([ps("psv%d" % i, [128, 8]) for i in range(2)])
            P.dma("sp", cv.t[:], cvec, dconst, writes=[cv.b], batch=True)
            P.dma("sp", bT.t[:], b_adaT, dconst, writes=[bT.b], batch=True)
            P.dma("sp", bgt.t[:], b_gtB, dconst, writes=[bgt.b], batch=True)
            P.dma("sp", g12.t[:], g12T, dconst, writes=[g12.b], batch=True)
            P.act(sv.t[:], cv.t[:], AF.Silu, reads=[cv.b], writes=[sv.b])
            P.cp("dve", srep.t[:], sv.t[:, :, 0:1].to_broadcast([128, KC, 128]), reads=[sv.b], writes=[srep.b])
            vec_of_block = {0: 0, 1: 0, 2: 1, 3: 1, 6: 2, 7: 2, 8: 3, 9: 3}
            for j in range(12):
                w = wst.next()
                P.dma(wst_q[wst.i], w.t[:], w_ada[:, j * 512:(j + 1) * 512].rearrange("(k p) c -> p k c", p=128),
                      wst_d[wst.i], writes=[w.b])
                half = j % 2
                if j in (4, 5, 10, 11):
                    gi = 0 if j < 6 else 1
                    pt = psg.next()
                    for kc in range(KC):
                        P.mm(pt.t[:, :], srep.t[:, kc, :], w.t[:, kc, :], start=(kc == 0), stop=(kc == KC - 1),
                             reads=[srep.b, w.b], writes=[pt.b])
                    P.tt("dve", gtB.t[:, gi, half * 512:(half + 1) * 512], pt.t[:, :],
                         bgt.t[:, gi, half * 512:(half + 1) * 512], ALU.add, reads=[pt.b, bgt.b], writes=[gtB.b])
                else:
                    vi = vec_of_block[j]
                    pt = psv.next()
                    for cc in range(4):
                        for kc in range(KC):
                            P.mm(pt.t[:, cc * 2:cc * 2 + 2], w.t[:, kc, cc * 128:(cc + 1) * 128], sv.t[:, kc, :],
                                 start=(kc == 0), stop=(kc == KC - 1), reads=[w.b, sv.b], writes=[pt.b])
                    P.tt("dve", mv.t[:, vi, half * 4:half * 4 + 4, :],
                         pt.t[:, :].rearrange("p (a b) -> p a b", b=2),
                         bT.t[:, j * 4:j * 4 + 4].unsqueeze(2).to_broadcast([128, 4, 2]), ALU.add,
                         reads=[pt.b, bT.b], writes=[mv.b])
            P.stt("dve", modv.t[:, 0, :], mv.t[:, 1, :, 0], 1.0, g12.t[:, 0, :], ALU.add, ALU.mult,
                  reads=[mv.b, g12.b], writes=[modv.b])
            P.cp("dve", modv.t[:, 1, :], mv.t[:, 0, :, 0], reads=[mv.b], writes=[modv.b])
            P.stt("dve", modv.t[:, 2, :], mv.t[:, 1, :, 1], 1.0, g12.t[:, 0, :], ALU.add, ALU.mult,
                  reads=[mv.b, g12.b], writes=[modv.b])
            P.cp("dve", modv.t[:, 3, :], mv.t[:, 0, :, 1], reads=[mv.b], writes=[modv.b])
            P.stt("dve", modv.t[:, 4, :], mv.t[:, 3, :, 0], 1.0, g12.t[:, 1, :], ALU.add, ALU.mult,
                  reads=[mv.b, g12.b], writes=[modv.b])
            P.cp("dve", modv.t[:, 5, :], mv.t[:, 2, :, 0], reads=[mv.b], writes=[modv.b])
            with nc.Block() as block:
                P.emit_phase(block)
        if _STOP == 1:
            return nc

        def run_pipeline(tiles, order):
            K = len(order)
            N = len(tiles)
            for i in range(N + K - 1):
                for s in order:
                    t = i - s
                    if 0 <= t < N and tiles[t][s] is not None:
                        tiles[t][s]()

        with ExitStack() as es:
            def sb(name, shape, dt):
                return TB(es.enter_context(nc.sbuf_tensor("sb_" + name, list(shape), dt)), name)

            def ps(name, shape, dt=F32):
                return TB(es.enter_context(nc.psum_tensor("ps_" + name, list(shape), dt)), name)

            def ring(name, n, shape, dt):
                return Ring([sb("%s%d" % (name, i), shape, dt) for i in range(n)])

            def dring(n):
                return [new_dsem() for _ in range(n)]


            xt_r = ring("xt", 3, [128, D], F32)
            xt_d = dring(3)
            sqj = sb("sqj", [128, D], BF16)
            stat_r = ring("stat", 4, [128, 4], F32)
            xn_r = ring("xn", 2, [128, D], BF16)
            mtx_r = ring("mtx", 2, [128, D], F32)
            hT_r = ring("hT", 3, [128, KC, 128], BF16)
            hT_d = dring(3)
            hTs_d = dring(3)
            qk_r = ring("qk", 4, [128, 512], F32)
            v_r = ring("v_sb", 7, [128, 512], BF16)
            a_r = ring("a_sb", 3, [33, 128], BF16)
            e1_r = ring("e1", 2, [128, 512], F32)
            l_r = ring("l_bf", 2, [128, 512], BF16)
            Ek_r = ring("Ek", 3, [128, 512], F32)
            el_r = ring("elast", 6, [128, 4], F32)
            kst_r = ring("kst", 2, [128, 2, 128], BF16)
            kstm_r = ring("kstm", 3, [128, 2, 128], BF16)
            Sb = sb("Sb", [128, 2, 128], F32)
            Sf = sb("Sf", [128, 2, 128], F32)
            Sbbf_r = ring("Sbbf", 5, [128, 2, 128], BF16)
            Sbbf_d = dring(5)
            Sbbfs_d = dring(5)
            Sfbf_r = ring("Sfbf", 3, [128, 2, 128], BF16)
            B0 = ps("B0", [128, 1024], BF16)
            B1 = ps("B1", [128, 512])
            B2 = ps("B2", [128, 512])
            B3 = ps("B3", [128, 512])
            B4 = ps("B4", [128, 512])
            B5 = ps("B5", [128, 512])
            B6 = ps("B6", [128, 512])
            B7a = ps("B7a", [128, 512])
            B7b = B0

            for r_ in a_r.items:
                P.op("dve", lambda e, r_=r_: e.memset(r_.t[:], 1.0), writes=[r_.b])
            P.op("dve", lambda e: e.memset(Sb.t[:], 0.0), writes=[Sb.b])
            P.op("dve", lambda e: e.memset(Sf.t[:], 0.0), writes=[Sf.b])
            for r_ in Sfbf_r.items + Sbbf_r.items:
                P.op("dve", lambda e, r_=r_: e.memset(r_.t[:], 0.0), writes=[r_.b])

            class CX:
                pass

            def load_x(src_ap):
                xt = xt_r.next()
                P.dma("sp", xt.t[:], src_ap, xt_d[xt_r.i], writes=[xt.b])
                return xt

            def norm_act(xt):
                st = stat_r.next()
                P.act(sqj.t[:], xt.t[:], AF.Square, accum_out=st.t[:, 0:1], reads=[xt.b], writes=[st.b])
                P.act(st.t[:, 1:2], st.t[:, 0:1], AF.Ln, bias=EPS, scale=1.0 / D, reads=[st.b], writes=[st.b])
                P.act(st.t[:, 2:3], st.t[:, 1:2], AF.Exp, scale=-0.5, reads=[st.b], writes=[st.b])
                return st

            def norm_dve(xt, st, on_act=False):
                xn = xn_r.next()
                if on_act:
                    P.act(xn.t[:], xt.t[:], AF.Copy, scale=st.t[:, 2:3], reads=[xt.b, st.b], writes=[xn.b])
                else:
                    P.ts("dve", xn.t[:], xt.t[:], st.t[:, 2:3], None, ALU.mult, reads=[xt.b, st.b], writes=[xn.b])
                return xn

            def transpose_mod(xn, gi, hT):
                for kc in range(KC):
                    P.tr(B0.t[:, kc * 128:(kc + 1) * 128], xn.t[:, kc * 128:(kc + 1) * 128], ident.t[:],
                         reads=[xn.b, ident.b], writes=[B0.b])
                mx = mtx_r.next()
                P.tt("dve", mx.t[:].rearrange("p (k t) -> p k t", k=KC), B0.t[:, :].rearrange("p (k t) -> p k t", k=KC),
                     modv.t[:, gi, :].unsqueeze(2).to_broadcast([128, KC, 128]), ALU.mult,
                     reads=[B0.b, modv.b], writes=[mx.b])
                P.tt("pool", hT.t[:], mx.t[:].rearrange("p (k t) -> p k t", k=KC),
                     modv.t[:, gi + 1, :].unsqueeze(2).to_broadcast([128, KC, 128]), ALU.add,
                     reads=[mx.b, modv.b], writes=[hT.b])

            def proj_fm(hT, col0, ncol, out_ap, out_b):
                for kc in range(KC):
                    P.mm(out_ap, w_in_sb.t[:, kc, col0:col0 + ncol], hT.t[:, kc, :], start=(kc == 0),
                         stop=(kc == KC - 1), reads=[w_in_sb.b, hT.b], writes=[out_b])

            def proj_tm(hT, col0, out_ap, out_b):
                for kc in range(KC):
                    P.mm(out_ap, hT.t[:, kc, :], w_in_sb.t[:, kc, col0:col0 + 512], start=(kc == 0),
                         stop=(kc == KC - 1), reads=[w_in_sb.b, hT.b], writes=[out_b])

            def decay_mm(a, dirs, PB):
                P.mm(PB.t[:, :], a.t[0:33, :], wdec_sb.t[0:33, :], reads=[a.b, wdec_sb.b], writes=[PB.b])
                e1 = e1_r.next()
                lo, hi = (0, 512) if len(dirs) == 2 else (dirs[0] * 256, dirs[0] * 256 + 256)
                P.act(e1.t[:, lo:hi], PB.t[:, lo:hi], AF.Exp, scale=-1.0, reads=[PB.b], writes=[e1.b])
                l = l_r.next()
                P.act(l.t[:, lo:hi], e1.t[:, lo:hi], AF.Ln, bias=1.0, reads=[e1.b], writes=[l.b])
                return l, lo, hi

            def cumsum_mm(l, dirs):
                for d in dirs:
                    for c in range(2):
                        j = d * 2 + c
                        P.mm(B3.t[:, j * 128:(j + 1) * 128], l.t[:, j * 128:(j + 1) * 128], tri[d].t[:],
                             reads=[l.b, tri[d].b], writes=[B3.b])

            def kst_make(d, Ek, ksrc, k0):
                el = el_r.next()
                last = 127 if d == 0 else 0
                cs3 = B3.t[:, :].rearrange("p (j t) -> p j t", t=128)
                P.act(el.t[:, 0:2], cs3[:, 2 * d:2 * d + 2, last], AF.Exp, scale=-1.0 / 16, reads=[B3.b], writes=[el.b])
                kst = kst_r.next()
                for c in range(2):
                    P.stt("dve", kst.t[:, c, :], ksrc.t[:, k0 + c * 128:k0 + (c + 1) * 128], el.t[:, c:c + 1],
                          Ek.t[:, (d * 2 + c) * 128:(d * 2 + c + 1) * 128], ALU.mult, ALU.mult,
                          reads=[ksrc.b, el.b, Ek.b], writes=[kst.b])
                return el, kst

            def kst_transpose(kst, PB):
                for c in range(2):
                    P.tr(PB.t[:, c * 128:(c + 1) * 128], kst.t[:, c, :], ident.t[:], reads=[kst.b, ident.b],
                         writes=[PB.b])
                kstm = kstm_r.next()
                P.cp("act", kstm.t[:].rearrange("p c t -> p (c t)"), PB.t[:, 0:256], reads=[PB.b], writes=[kstm.b])
                return kstm

            def state_apply(el, kstm, v, S, Sbf_ring, PB):
                for c in range(2):
                    P.mm(PB.t[:, c * 256:(c + 1) * 256], kstm.t[:, c, :], v.t[:, c * 256:(c + 1) * 256],
                         reads=[kstm.b, v.b], writes=[PB.b])
                for c in range(2):
                    for hf in range(2):
                        r0 = hf * 64
                        P.stt("dve", S.t[r0:r0 + 64, c, :], S.t[r0:r0 + 64, c, :], el.t[r0:r0 + 64, c:c + 1],
                              PB.t[r0:r0 + 64, c * 256 + hf * 128:c * 256 + (hf + 1) * 128], ALU.mult, ALU.add,
                              reads=[S.b, el.b, PB.b], writes=[S.b])
                sbf = Sbf_ring.next()
                P.cp("pool", sbf.t[:], S.t[:], reads=[S.b], writes=[sbf.b])
                return sbf

            with ExitStack() as esa:
                def sba(name, shape, dt):
                    return TB(esa.enter_context(nc.sbuf_tensor("sa_" + name, list(shape), dt)), name)

                NP = 14
                p_r = [sba("p_sb%d" % i, [128, 512], BF16) for i in range(NP)]
                diff_r = Ring([sba("diff%d" % i, [128, 4, 128], BF16) for i in range(2)])
                pl_r = Ring([sba("plbf%d" % i, [128, 4, 128], BF16) for i in range(2)])
                pl_d = dring(2)
                wpool_sb = sba("wpool_sb", [128, 4, 128], BF16)
                A_sb = sba("A_sb", [128, NBLK_A, 128], BF16)
                dw2 = new_dsem()
                P.dma("pool", wpool_sb.t[:], w_pool.rearrange("g c d -> c g d"), dw2, writes=[wpool_sb.b], batch=True)
                for b0 in range(0, NBLK_A, 4):
                    b1 = min(NBLK_A, b0 + 4)
                    P.dma("pool", A_sb.t[:, b0:b1, :], apool[b0:b1].rearrange("n k t -> k n t"), dw2, writes=[A_sb.b],
                          batch=True)

                def slot_of(tile):
                    return tile % NP

                def pool_fin1(tt):
                    for gi, R in enumerate(POOL_R):
                        v_ = tt if tt < R else R
                        dts = [dt for dt in range(-R, R + 1) if tt + dt >= 0]
                        for n_, dt in enumerate(dts):
                            pt = p_r[slot_of(tt + dt)]
                            blk = POOL_TAB[(v_, gi, dt)]
                            P.mm(B2.t[:, gi * 128:(gi + 1) * 128], pt.t[:, gi * 128:(gi + 1) * 128], A_sb.t[:, blk, :],
                                 start=(n_ == 0), stop=(n_ == len(dts) - 1), reads=[pt.b, A_sb.b], writes=[B2.b])
                    df = diff_r.next()
                    P.cp("act", df.t[:].rearrange("p g t -> p (g t)"), B2.t[:, :], reads=[B2.b], writes=[df.b])
                    return df

                def pool_fin2(tt, df):
                    for gi in range(4):
                        P.mm(B6.t[:, gi * 128:(gi + 1) * 128], wpool_sb.t[:, gi, :], df.t[:, gi, :],
                             reads=[wpool_sb.b, df.b], writes=[B6.b])
                    pl = pl_r.next()
                    P.tt("dve", pl.t[:], B6.t[:, :].rearrange("p (g t) -> p g t", g=4),
                         psc.t[:, :].unsqueeze(2).to_broadcast([128, 4, 128]), ALU.mult, reads=[B6.b, psc.b], writes=[pl.b])
                    P.dma("pool", pl_scr[tt], pl.t[:], pl_d[pl_r.i], reads=[pl.b], writes=[pl_b[tt]])

                def a_tile(src_ap, gi, d, S, Sring, own_tau, pool_tau, fin_tt):
                    cx = CX()

                    def s0():
                        cx.xt = load_x(src_ap)
                        cx.st = norm_act(cx.xt)

                    def s1():
                        cx.xn = norm_dve(cx.xt, cx.st)

                    def s2():
                        cx.hT = hT_r.next()
                        transpose_mod(cx.xn, gi, cx.hT)
                        if own_tau is not None:
                            P.dma("pool", hT_scr[own_tau], cx.hT.t[:], hTs_d[hT_r.i], reads=[cx.hT.b],
                                  writes=[hT_b[own_tau]])

                    def s3():
                        hT = cx.hT
                        proj_fm(hT, OFF_K, 128, B1.t[:, 0:128], B1.b)
                        proj_fm(hT, OFF_K + 128, 128, B1.t[:, 128:256], B1.b)
                        proj_tm(hT, OFF_V, B2.t[:, :], B2.b)
                        proj_fm(hT, OFF_A, 32, B7a.t[0:32, 0:128], B7a.b)
                        cx.kq = qk_r.next()
                        P.cp("dve", cx.kq.t[:, 0:256], B1.t[:, 0:256], reads=[B1.b], writes=[cx.kq.b])
                        cx.v = v_r.next()
                        P.cp("act", cx.v.t[:], B2.t[:, :], reads=[B2.b], writes=[cx.v.b])
                        cx.a = a_r.next()
                        P.cp("act", cx.a.t[0:32, :], B7a.t[0:32, 0:128], reads=[B7a.b], writes=[cx.a.b])
                        if pool_tau is not None:
                            proj_tm(hT, OFF_P, B6.t[:, :], B6.b)
                            pt = p_r[slot_of(pool_tau)]
                            P.cp("dve", pt.t[:], B6.t[:, :], reads=[B6.b], writes=[pt.b])

                    def s4():
                        cx.l, cx.lo, cx.hi = decay_mm(cx.a, [d], B4)

                    def s5():
                        cumsum_mm(cx.l, [d])
                        Ek = cx.Ek = Ek_r.next()
                        P.act(Ek.t[:, cx.lo:cx.hi], B3.t[:, cx.lo:cx.hi], AF.Exp, scale=1.0 / 16, reads=[B3.b], writes=[Ek.b])
                        el = cx.el = el_r.next()
                        last = 127 if d == 0 else 0
                        cs3 = B3.t[:, :].rearrange("p (j t) -> p j t", t=128)
                        P.act(el.t[:, 0:2], cs3[:, 2 * d:2 * d + 2, last], AF.Exp, scale=-1.0 / 16, reads=[B3.b], writes=[el.b])

                    def s6():
                        kst = cx.kst = kst_r.next()
                        for c in range(2):
                            P.stt("dve", kst.t[:, c, :], cx.kq.t[:, c * 128:(c + 1) * 128], cx.el.t[:, c:c + 1],
                                  cx.Ek.t[:, (d * 2 + c) * 128:(d * 2 + c + 1) * 128], ALU.mult, ALU.mult,
                                  reads=[cx.kq.b, cx.el.b, cx.Ek.b], writes=[kst.b])

                    def s7():
                        cx.kstm = kst_transpose(cx.kst, B7b)

                    def s8():
                        if own_tau is not None:
                            sbf = Sring.cur()
                            P.dma("pool", S_scr[own_tau], sbf.t[:], Sbbfs_d[Sring.i], reads=[sbf.b], writes=[S_b[own_tau]])
                        state_apply(cx.el, cx.kstm, cx.v, S, Sring, B5)
                        if fin_tt is not None:
                            cx.df = pool_fin1(fin_tt)

                    def s9():
                        if fin_tt is not None:
                            pool_fin2(fin_tt, cx.df)

                    return [s0, s1, s2, s3, s4, s5, s6, s7, s8, s9]

                def fin_tile(tt):
                    cx = CX()

                    def s8():
                        cx.df = pool_fin1(tt)

                    def s9():
                        pool_fin2(tt, cx.df)

                    return [None] * 8 + [s8, s9]

                tilesA = []
                if NCTX == 1:
                    raise NotImplementedError
                for i in range(NCTX):
                    tilesA.append(a_tile(ctxs[i * 128:(i + 1) * 128, :], 2, 0, Sf, Sfbf_r, None, None, None))
                for i in reversed(range(NCTX)):
                    tilesA.append(a_tile(ctxs[i * 128:(i + 1) * 128, :], 2, 1, Sb, Sbbf_r, None, None, None))
                for tau in reversed(range(NT_ALL)):
                    tt = tau + HALO
                    tilesA.append(a_tile(xs[tau * 128:(tau + 1) * 128, :], 0, 1, Sb, Sbbf_r,
                                         tau if tau < NT_OWN else None,
                                         tau if tau < NT_OWN + HALO else None,
                                         tt if tt < NT_OWN else None))
                for tt in reversed(range(min(HALO, NT_OWN))):
                    tilesA.append(fin_tile(tt))
                run_pipeline(tilesA, [9, 8, 7, 2, 6, 5, 4, 3, 1, 0])
                with nc.Block() as block:
                    P.emit_phase(block)
            if _STOP == 4:
                return nc

            with ExitStack() as esb:
                def sbb(name, shape, dt):
                    return TB(esb.enter_context(nc.sbuf_tensor("sbb_" + name, list(shape), dt)), name)

                def ringb(name, n, shape, dt):
                    return Ring([sbb("%s%d" % (name, i), shape, dt) for i in range(n)])

                w_out_sb = sbb("w_out_sb", [128, KC, D], BF16)
                w_r_sb = sbb("w_r_sb", [128, KC, 36], BF16)
                dw3 = new_dsem()
                for kc in range(KC):
                    P.dma("pool", w_out_sb.t[:, kc, :], w_out[kc * 128:(kc + 1) * 128, :], dw3, writes=[w_out_sb.b], batch=True)
                P.dma("pool", w_r_sb.t[:], w_r.rearrange("(k p) c -> p k c", p=128), dw3, writes=[w_r_sb.b], batch=True)
                Eq_r = ringb("Eq", 2, [128, 512], F32)
                qb_r = ringb("qb", 3, [128, 2, 2, 2, 128], BF16)
                kb_r = ringb("kb", 2, [128, 4, 128], BF16)
                scm_r = ringb("scm", 2, [128, 8, 128], BF16)
                sg_r = ringb("sg", 6, [128, 512], BF16)
                sl_r = ringb("sl", 2, [128, 512], F32)
                ot_r = ringb("ot", 2, [128, 512], F32)
                mtm_r = ringb("mtm", 3, [128, 512], BF16)
                mixT_r = ringb("mixT", 3, [128, KC, 128], BF16)
                mixT_d = dring(3)
                mt_r = ringb("mt", 2, [128, D], F32)
                x1_r = ringb("x1", 3, [128, D], F32)
                x1_d = dring(3)
                h2_r = ringb("h2T", 3, [128, KC, 128], BF16)
                h2_d = dring(3)
                RG = 4
                assert NT_OWN % RG == 0
                rl_r = ringb("rl", 2, [128, RG, 36], F32)
                rw_r = ringb("rw", 2, [128, RG, 160], F32)
                rstate = {}
                for r_ in qb_r.items:
                    P.op("dve", lambda e, r_=r_: e.memset(r_.t[:], 0.0), writes=[r_.b])

                def b_tile(t):
                    cx = CX()

                    def s0():
                        cx.hT = hT_r.next()
                        P.dma("sp", cx.hT.t[:], hT_scr[t], hT_d[hT_r.i], reads=[hT_b[t]], writes=[cx.hT.b])

                    def s1():
                        hT = cx.hT
                        for c in range(4):
                            proj_fm(hT, c * 128, 128, B1.t[:, c * 128:(c + 1) * 128], B1.b)
                        proj_tm(hT, OFF_V, B2.t[:, :], B2.b)
                        proj_fm(hT, OFF_A, 32, B7a.t[0:32, 0:128], B7a.b)
                        proj_tm(hT, OFF_G, B6.t[:, :], B6.b)
                        cx.qk = qk_r.next()
                        P.cp("act", cx.qk.t[:], B1.t[:, :], reads=[B1.b], writes=[cx.qk.b])
                        cx.v = v_r.next()
                        P.cp("act", cx.v.t[:], B2.t[:, :], reads=[B2.b], writes=[cx.v.b])
                        cx.a = a_r.next()
                        P.cp("act", cx.a.t[0:32, :], B7a.t[0:32, 0:128], reads=[B7a.b], writes=[cx.a.b])
                        cx.sl = sl_r.next()
                        P.act(cx.sl.t[:], B6.t[:, :], AF.Silu, reads=[B6.b], writes=[cx.sl.b])

                    def s2():
                        cx.sg = sg_r.next()
                        P.tt("pool", cx.sg.t[:].rearrange("p (h e) -> p h e", h=4), cx.sl.t[:].rearrange("p (h e) -> p h e", h=4),
                             ggB.t[:, :].unsqueeze(1).to_broadcast([128, 4, 128]), ALU.mult, reads=[cx.sl.b, ggB.b],
                             writes=[cx.sg.b])
                        cx.l, _, _ = decay_mm(cx.a, [0, 1], B7a)

                    def s3():
                        cumsum_mm(cx.l, [0, 1])
                        Ek = cx.Ek = Ek_r.next()
                        Eq = cx.Eq = Eq_r.next()
                        P.act(Ek.t[:, :], B3.t[:, :], AF.Exp, scale=1.0 / 16, reads=[B3.b], writes=[Ek.b])
                        P.act(Eq.t[:, :], B3.t[:, :], AF.Exp, scale=-1.0 / 16, reads=[B3.b], writes=[Eq.b])
                        el = cx.el = el_r.next()
                        cs3 = B3.t[:, :].rearrange("p (j t) -> p j t", t=128)
                        P.act(el.t[:, 0:2], cs3[:, 0:2, 127], AF.Exp, scale=-1.0 / 16, reads=[B3.b], writes=[el.b])
                        cx.sbin = Sbbf_r.next()
                        P.dma("sp", cx.sbin.t[:], S_scr[t], Sbbf_d[Sbbf_r.i], reads=[S_b[t]], writes=[cx.sbin.b])

                    def s4():
                        qk, Ek, Eq, el = cx.qk, cx.Ek, cx.Eq, cx.el
                        kst = cx.kst = kst_r.next()
                        for c in range(2):
                            P.stt("dve", kst.t[:, c, :], qk.t[:, 256 + c * 128:256 + (c + 1) * 128], el.t[:, c:c + 1],
                                  Ek.t[:, c * 128:(c + 1) * 128], ALU.mult, ALU.mult,
                                  reads=[qk.b, el.b, Ek.b], writes=[kst.b])
                        qb = cx.qb = qb_r.next()
                        kb = cx.kb = kb_r.next()
                        for hp in range(2):
                            r0 = hp * 64
                            P.stt("dve", qb.t[r0:r0 + 64, :, :, hp, :],
                                  qk.t[r0:r0 + 64, 0:256].rearrange("p (c t) -> p c t", c=2).unsqueeze(1).to_broadcast([64, 2, 2, 128]),
                                  0.125, Eq.t[r0:r0 + 64, :].rearrange("p (d c t) -> p d c t", d=2, c=2), ALU.mult, ALU.mult,
                                  reads=[qk.b, Eq.b], writes=[qb.b])
                        P.tt("dve", kb.t[:].rearrange("p (d c) t -> p d (c t)", d=2),
                             qk.t[:, 256:512].unsqueeze(1).to_broadcast([128, 2, 256]),
                             Ek.t[:, :].rearrange("p (d x) -> p d x", d=2), ALU.mult, reads=[qk.b, Ek.b], writes=[kb.b])

                    def s5():
                        cx.kstm = kst_transpose(cx.kst, B0)
                        kb, qb = cx.kb, cx.qb
                        scb = (B4, B5)
                        for d in range(2):
                            for h in range(4):
                                c = h // 2
                                P.mm(scb[d].t[:, h * 128:(h + 1) * 128], kb.t[:, d * 2 + c, :],
                                     qb.t[:, d, c, h % 2, :], reads=[kb.b, qb.b], writes=[scb[d].b])
                        scm = cx.scm = scm_r.next()
                        for d in range(2):
                            P.tt("dve", scm.t[:, d * 4:(d + 1) * 4, :], scb[d].t[:, :].rearrange("p (h t) -> p h t", h=4),
                                 tri[d].t[:, :].unsqueeze(1).to_broadcast([128, 4, 128]), ALU.mult,
                                 reads=[scb[d].b, tri[d].b], writes=[scm.b])

                    def s6():
                        scm, v, qb, sbin = cx.scm, cx.v, cx.qb, cx.sbin
                        sfbf = Sfbf_r.cur()
                        for h in range(4):
                            c = h // 2
                            oo = B6.t[:, h * 128:(h + 1) * 128]
                            P.mm(oo, scm.t[:, h, :], v.t[:, h * 128:(h + 1) * 128], start=True, stop=False,
                                 reads=[scm.b, v.b], writes=[B6.b])
                            P.mm(oo, scm.t[:, 4 + h, :], v.t[:, h * 128:(h + 1) * 128], start=False, stop=False,
                                 reads=[scm.b, v.b], writes=[B6.b])
                            P.mm(oo, qb.t[:, 1, c, h % 2, :], sbin.t[:, c, :], start=False, stop=False,
                                 reads=[qb.b, sbin.b], writes=[B6.b])
                            P.mm(oo, qb.t[:, 0, c, h % 2, :], sfbf.t[:, c, :], start=False, stop=True,
                                 reads=[qb.b, sfbf.b], writes=[B6.b])
                        st = stat_r.next()
                        for h in range(4):
                            P.act(sqj.t[:, h * 128:(h + 1) * 128], B6.t[:, h * 128:(h + 1) * 128], AF.Square,
                                  accum_out=st.t[:, h:h + 1], reads=[B6.b], writes=[st.b])
                        P.act(st.t[:, 0:4], st.t[:, 0:4], AF.Ln, bias=EPS, scale=1.0 / 128, reads=[st.b], writes=[st.b])
                        P.act(st.t[:, 0:4], st.t[:, 0:4], AF.Exp, scale=-0.5, reads=[st.b], writes=[st.b])
                        ot = cx.ot = ot_r.next()
                        P.tt("dve", ot.t[:].rearrange("p (h e) -> p h e", h=4), B6.t[:, :].rearrange("p (h e) -> p h e", h=4),
                             st.t[:, 0:4].unsqueeze(2).to_broadcast([128, 4, 128]), ALU.mult, reads=[B6.b, st.b], writes=[ot.b])

                    def s7():
                        state_apply(cx.el, cx.kstm, cx.v, Sf, Sfbf_r, B3)
                        cx.mtm = mtm_r.next()
                        P.tt("pool", cx.mtm.t[:], cx.ot.t[:], cx.sg.t[:], ALU.mult, reads=[cx.ot.b, cx.sg.b], writes=[cx.mtm.b])
                        cx.mixT = mixT_r.next()
                        P.dma("sp", cx.mixT.t[:, 4:8, :], pl_scr[t], mixT_d[mixT_r.i], reads=[pl_b[t]], writes=[cx.mixT.b])

                    def s8():
                        mtm, mixT = cx.mtm, cx.mixT
                        for h in range(4):
                            P.tr(B0.t[:, h * 128:(h + 1) * 128], mtm.t[:, h * 128:(h + 1) * 128], ident.t[:],
                                 reads=[mtm.b, ident.b], writes=[B0.b])
                        P.cp("act", mixT.t[:, 0:4, :].rearrange("p k t -> p (k t)"), B0.t[:, 0:512], reads=[B0.b],
                             writes=[mixT.b])
                        cx.xt = load_x(xs[t * 128:(t + 1) * 128, :])

                    def s9():
                        mixT, xt = cx.mixT, cx.xt
                        ob = (B1, B2)
                        for half in range(2):
                            for kc in range(KC):
                                P.mm(ob[half].t[:, :], mixT.t[:, kc, :], w_out_sb.t[:, kc, half * 512:(half + 1) * 512],
                                     start=(kc == 0), stop=(kc == KC - 1), reads=[mixT.b, w_out_sb.b], writes=[ob[half].b])
                        x1 = cx.x1 = x1_r.next()
                        mt = mt_r.next()
                        for half in range(2):
                            P.tt("dve", mt.t[:, half * 512:(half + 1) * 512], ob[half].t[:, :],
                                 gtB.t[:, 0, half * 512:(half + 1) * 512], ALU.mult, reads=[ob[half].b, gtB.b], writes=[mt.b])
                        P.tt("pool", x1.t[:], mt.t[:], xt.t[:], ALU.add, reads=[mt.b, xt.b], writes=[x1.b])
                        P.dma("pool", y[t * 128:(t + 1) * 128, :], x1.t[:], x1_d[x1_r.i], reads=[x1.b], writes=[y_b[t]])

                    def s10():
                        cx.st = norm_act(cx.x1)

                    def s10b():
                        cx.xn = norm_dve(cx.x1, cx.st, on_act=True)

                    def s11():
                        cx.h2 = h2_r.next()
                        transpose_mod(cx.xn, 4, cx.h2)
                        P.dma("pool", h2_scr[:, :, t * 128:(t + 1) * 128], cx.h2.t[:], h2_d[h2_r.i], reads=[cx.h2.b],
                              writes=[h2_b[t]])

                    def s12():
                        h2 = cx.h2
                        for kc in range(KC):
                            P.mm(B7a.t[:, 128:164], h2.t[:, kc, :], w_r_sb.t[:, kc, :], start=(kc == 0), stop=(kc == KC - 1),
                                 reads=[h2.b, w_r_sb.b], writes=[B7a.b])
                        if t % RG == 0:
                            rstate["L"] = rl_r.next()
                        L = rstate["L"]
                        cx.L = L
                        P.cp("dve", L.t[:, t % RG, :], B7a.t[:, 128:164], reads=[B7a.b], writes=[L.b])

                    def r13():
                        L = cx.L
                        Wt = cx.W = rw_r.next()
                        W = Wt.t
                        rb = [Wt.b]
                        lg = L.t[:, :, 0:4]
                        gmax_b = W[:, :, 4:5].to_broadcast([128, RG, 4])
                        P.op("dve", lambda e: e.tensor_reduce(W[:, :, 4:5], lg, AX.X, ALU.max), reads=[L.b], writes=rb)
                        P.tt("dve", W[:, :, 8:12], lg, gmax_b, ALU.is_equal, reads=[L.b] + rb, writes=rb)
                        P.tt("dve", W[:, :, 12:16], lg, gmax_b, ALU.subtract, reads=[L.b] + rb, writes=rb)
                        P.act(W[:, :, 16:20], W[:, :, 12:16], AF.Exp, reads=rb, writes=rb)

                    def r14():
                        L = cx.L
                        W = cx.W.t
                        rb = [cx.W.b]
                        le4 = L.t[:, :, 4:36].rearrange("p g (a j) -> p g a j", a=4)
                        P.op("dve", lambda e: e.tensor_reduce(W[:, :, 5:6], W[:, :, 16:20], AX.X, ALU.add), reads=rb, writes=rb)
                        P.op("dve", lambda e: e.reciprocal(W[:, :, 6:7], W[:, :, 5:6]), reads=rb, writes=rb)
                        P.ts("dve", W[:, :, 20:24], W[:, :, 8:12], 1.0, BIG, ALU.subtract, ALU.mult, reads=rb, writes=rb)
                        P.tt("dve", W[:, :, 32:64].rearrange("p g (a j) -> p g a j", a=4), le4,
                             W[:, :, 20:24].unsqueeze(3).to_broadcast([128, RG, 4, 8]), ALU.add, reads=[L.b] + rb, writes=rb)
                        P.op("dve", lambda e: e.tensor_reduce(W[:, :, 24:25], W[:, :, 32:64], AX.X, ALU.max), reads=rb, writes=rb)
                        P.tt("dve", W[:, :, 64:96], W[:, :, 32:64], W[:, :, 24:25].to_broadcast([128, RG, 32]), ALU.is_equal,
                             reads=rb, writes=rb)
                        P.stt("dve", W[:, :, 96:128], W[:, :, 64:96], -BIG, W[:, :, 32:64], ALU.mult, ALU.add, reads=rb, writes=rb)
                        P.op("dve", lambda e: e.tensor_reduce(W[:, :, 25:26], W[:, :, 96:128], AX.X, ALU.max), reads=rb, writes=rb)
                        P.tt("dve", W[:, :, 128:160], W[:, :, 96:128], W[:, :, 25:26].to_broadcast([128, RG, 32]), ALU.is_equal,
                             reads=rb, writes=rb)
                        P.tt("dve", W[:, :, 26:27], W[:, :, 25:26], W[:, :, 24:25], ALU.subtract, reads=rb, writes=rb)
                        P.act(W[:, :, 27:28], W[:, :, 26:27], AF.Exp, reads=rb, writes=rb)

                    def r15():
                        W = cx.W.t
                        rb = [cx.W.b]
                        P.ts("dve", W[:, :, 0:1], W[:, :, 27:28], 1.0, None, ALU.add, reads=rb, writes=rb)
                        P.op("dve", lambda e: e.reciprocal(W[:, :, 1:2], W[:, :, 0:1]), reads=rb, writes=rb)
                        P.tt("dve", W[:, :, 2:3], W[:, :, 1:2], W[:, :, 6:7], ALU.mult, reads=rb, writes=rb)
                        P.tt("dve", W[:, :, 3:4], W[:, :, 2:3], W[:, :, 27:28], ALU.mult, reads=rb, writes=rb)
                        P.tt("dve", W[:, :, 32:64], W[:, :, 64:96], W[:, :, 2:3].to_broadcast([128, RG, 32]), ALU.mult,
                             reads=rb, writes=rb)
                        P.tt("dve", W[:, :, 96:128], W[:, :, 128:160], W[:, :, 3:4].to_broadcast([128, RG, 32]), ALU.mult,
                             reads=rb, writes=rb)
                        P.tt("dve", gates.t[:, t - RG + 1:t + 1, :], W[:, :, 32:64], W[:, :, 96:128], ALU.add,
                             reads=rb, writes=[gates.b])

                    if t % RG != RG - 1:
                        r13 = r14 = r15 = None
                    return [s0, s1, s2, s3, s4, s5, s6, s7, s8, s9, s10, s10b, s11, s12, r13, r14, r15]

                tilesB = [b_tile(t) for t in range(NT_OWN)]
                run_pipeline(tilesB, [7, 12, 11, 10, 9, 8, 6, 5, 4, 3, 2, 1, 0, 16, 15, 14, 13])
                with nc.Block() as block:
                    P.emit_phase(block)
        if _STOP == 2:
            return nc
        wes.close()

        with ExitStack() as es:
            def sb(name, shape, dt):
                return TB(es.enter_context(nc.sbuf_tensor("sb_" + name, list(shape), dt)), name)

            def ps(name, shape, dt=F32):
                return TB(es.enter_context(nc.psum_tensor("ps_" + name, list(shape), dt)), name)

            NSUB = TPB // 4 if TPB >= 4 else 1
            TPS = TPB // NSUB
            h2sub = [sb("h2blk%d" % i, [128, KC, TPS * 128], BF16) for i in range(NSUB)]
            dh2 = [new_dsem() for _ in range(NSUB)]
            yacc = [sb("yacc%d" % i, [128, D], F32) for i in range(TPB)]
            wi_r = Ring([sb("wi%d" % i, [128, KC, 2 * DEXP], BF16) for i in range(2)])
            wo_r = Ring([sb("wo%d" % i, [128, 2, D], BF16) for i in range(2)])
            we_d = [new_dsem(), new_dsem()]
            sa_r = Ring([sb("sa%d" % i, [128, 512], F32) for i in range(2)])
            gT_r = Ring([sb("gT%d" % i, [128, 2, 512], BF16) for i in range(3)])
            x1t_r = Ring([sb("x1t%d" % i, [128, D], F32) for i in range(5)])
            x1t_d = [new_dsem() for _ in range(5)]
            sqj2 = sb("sqj2", [128, D], BF16)
            st2_r = Ring([sb("st2%d" % i, [128, 4], F32) for i in range(4)])
            yo_r = Ring([sb("yo%d" % i, [128, D], F32) for i in range(3)])
            yo_d = [new_dsem() for _ in range(3)]
            pa = [ps("pa%d" % q, [128, 512]) for q in range(2)]
            pu = [ps("pu%d" % q, [128, 512]) for q in range(2)]
            po_r = Ring([(ps("po%da" % i, [128, 512]), ps("po%db" % i, [128, 512])) for i in range(2)])
            out_dsems = yo_d

            def load_h2(blk, sub):
                tok0 = (blk * TPB + sub * TPS) * 128
                P.dma("sp", h2sub[sub].t[:], h2_scr[:, :, tok0:tok0 + TPS * 128], dh2[sub],
                      reads=[h2_b[blk * TPB + sub * TPS + i] for i in range(TPS)], writes=[h2sub[sub].b])

            fin_tiles = []

            NFS = 4

            def fin_stage(ft):
                stage, blk, ti, cx = ft
                tg = blk * TPB + ti
                if stage == 0:
                    x1t = cx["x1t"] = x1t_r.next()
                    P.dma("sp", x1t.t[:], y[tg * 128:(tg + 1) * 128, :], x1t_d[x1t_r.i], reads=[y_b[tg]], writes=[x1t.b])
                elif stage == 1:
                    x1t = cx["x1t"]
                    P.tt("pool", x1t.t[:], x1t.t[:], yacc[ti].t[:], ALU.add, reads=[x1t.b, yacc[ti].b], writes=[x1t.b])
                elif stage == 2:
                    x1t = cx["x1t"]
                    st = cx["st"] = st2_r.next()
                    P.act(sqj2.t[:], x1t.t[:], AF.Square, accum_out=st.t[:, 0:1], reads=[x1t.b], writes=[st.b])
                    P.act(st.t[:, 1:2], st.t[:, 0:1], AF.Ln, bias=EPS, scale=1.0 / D, reads=[st.b], writes=[st.b])
                    P.act(st.t[:, 2:3], st.t[:, 1:2], AF.Exp, scale=-0.5, reads=[st.b], writes=[st.b])
                else:
                    x1t, st = cx["x1t"], cx["st"]
                    yo = yo_r.next()
                    P.stt("dve", yo.t[:], x1t.t[:], st.t[:, 2:3], fgB.t[:], ALU.mult, ALU.mult,
                          reads=[x1t.b, st.b, fgB.b], writes=[yo.b])
                    P.dma("sp", y[tg * 128:(tg + 1) * 128, :], yo.t[:], yo_d[yo_r.i], reads=[yo.b], writes=[y_b[tg]])
                ft[0] = stage + 1

            def fin_step():
                todo = [ft for ft in fin_tiles if ft[0] < NFS][:NFS]
                for ft in todo:
                    fin_stage(ft)
                return len(todo) > 0

            def fin_require(blk, ti):
                for ft in fin_tiles:
                    if ft[1] == blk and ft[2] == ti:
                        while ft[0] < 2:
                            fin_step()

            def moe_down(blk, e_, sub, gT, wo, part=None):
                hs = max(1, TPS // 2)
                jr = range(TPS) if part is None else (range(0, hs) if part == 0 else range(hs, TPS))
                last_part = part is None or part == 1
                for j in jr:
                    ti = sub * TPS + j
                    tg = blk * TPB + ti
                    po = po_r.next()
                    for half in range(2):
                        for q in range(2):
                            P.mm(po[half].t[:, :], gT.t[:, q, j * 128:(j + 1) * 128],
                                 wo.t[:, q, half * 512:(half + 1) * 512], start=(q == 0), stop=(q == 1),
                                 reads=[gT.b, wo.b], writes=[po[half].b])
                    if e_ == 0 and blk > 0:
                        fin_require(blk - 1, ti)
                    for half in range(2):
                        ya = yacc[ti].t[:, half * 512:(half + 1) * 512]
                        if e_ == 0:
                            P.ts("dve", ya, po[half].t[:, :], gates.t[:, tg, e_:e_ + 1], None, ALU.mult,
                                 reads=[po[half].b, gates.b], writes=[yacc[ti].b])
                        else:
                            P.stt("dve", ya, po[half].t[:, :], gates.t[:, tg, e_:e_ + 1], ya, ALU.mult, ALU.add,
                                  reads=[po[half].b, gates.b, yacc[ti].b], writes=[yacc[ti].b])
                if not last_part:
                    return
                if e_ == NEXP - 1:
                    for j in range(TPS):
                        fin_tiles.append([0, blk, sub * TPS + j, {}])
                for _ in range(4):
                    fin_step()

            pending = None
            for sub in range(NSUB):
                load_h2(0, sub)
            seq = [(b_, e_) for b_ in range(NBLK) for e_ in range(NEXP)]

            def issue_w(idx):
                e_ = seq[idx][1]
                wi = wi_r.next()
                wo = wo_r.next()
                P.dma("pool", wi.t[:], w_ei[e_].rearrange("(k p) c -> p k c", p=128), we_d[wi_r.i], writes=[wi.b])
                P.dma("pool", wo.t[:], w_eo[e_].rearrange("(k p) c -> p k c", p=128), we_d[wi_r.i], writes=[wo.b],
                      batch=True)
                P.tt("pool", wo.t[:], wo.t[:], gtB.t[:, 1, :].unsqueeze(1).to_broadcast([128, 2, D]), ALU.mult,
                     reads=[wo.b, gtB.b], writes=[wo.b])
                return wi, wo

            wcur = issue_w(0)
            if True:
                for idx, (blk, e_) in enumerate(seq):
                    wi, wo = wcur
                    for sub in range(NSUB):
                        ns = TPS * 128
                        hb = h2sub[sub]
                        gT = gT_r.next()
                        for q in range(2):
                            for kc in range(KC):
                                P.mm(pa[q].t[:, 0:ns], wi.t[:, kc, q * 128:(q + 1) * 128], hb.t[:, kc, :],
                                     start=(kc == 0), stop=(kc == KC - 1), reads=[wi.b, hb.b], writes=[pa[q].b])
                            for kc in range(KC):
                                P.mm(pu[q].t[:, 0:ns], wi.t[:, kc, DEXP + q * 128:DEXP + (q + 1) * 128],
                                     hb.t[:, kc, :], start=(kc == 0), stop=(kc == KC - 1),
                                     reads=[wi.b, hb.b], writes=[pu[q].b])
                            sa = sa_r.next()
                            P.act(sa.t[:, 0:ns], pa[q].t[:, 0:ns], AF.Silu, reads=[pa[q].b], writes=[sa.b])
                            P.tt("dve", gT.t[:, q, 0:ns], sa.t[:, 0:ns], pu[q].t[:, 0:ns], ALU.mult,
                                 reads=[sa.b, pu[q].b], writes=[gT.b])
                            if pending is not None:
                                moe_down(*pending, part=q)
                        if e_ == NEXP - 1 and blk + 1 < NBLK:
                            load_h2(blk + 1, sub)
                        pending = (blk, e_, sub, gT, wo)
                        if sub == 0 and idx + 1 < len(seq):
                            wcur = issue_w(idx + 1)
            if pending is not None:
                moe_down(*pending)
                pending = None
            while fin_step():
                pass
            with nc.Block() as block:
                P.emit_phase(block, final=out_dsems)
    return nc


def _win_matrix(n, w):
    lo = w // 2
    hi = w - 1 - lo
    M = np.zeros((n, n), np.float32)
    for i in range(n):
        s = min(max(i - lo, 0), n)
        e = min(max(i + hi + 1, 0), n)
        M[i, s:e] = 1.0 / float(e - s)
    return M


def _pool_blocks(rows_total, par):
    blocks = np.zeros((NBLK_A, 128, 128), np.float32)
    eye = np.eye(128, dtype=np.float32)
    for gi, (w, R) in enumerate(zip(POOL_W, POOL_R)):
        Mr = _win_matrix(rows_total, w)
        Mc = _win_matrix(GRID_W, w)
        if par:
            Mr = Mr[::-1, ::-1]
            Mc = Mc[::-1, ::-1]
        Acol = np.ascontiguousarray(Mc.T)
        for v in range(R + 1):
            t = v
            for dt in range(-R, R + 1):
                if (v, gi, dt) not in POOL_TAB:
                    continue
                blk = np.zeros((2, GRID_W, 2, GRID_W), np.float32)
                for a in range(2):
                    r_in = 2 * (t + dt) + a
                    for b2 in range(2):
                        r_out = 2 * t + b2
                        if r_in < rows_total and r_out < rows_total:
                            blk[a, :, b2, :] = Mr[r_out, r_in] * Acol
                blk = blk.reshape(128, 128)
                if dt == 0:
                    blk = blk - eye
                blocks[POOL_TAB[(v, gi, dt)]] = blk
    return blocks


_PROG_CACHE = {}


def kernel(x, c, ctx, c_ctx, w_ada, b_ada, norm1_g, w_in, w_decay, b_decay, gla_norm_g, w_pool, pool_scale,
           w_out, norm2_g, w_router_group, w_router_expert, w_expert_in, w_expert_out, final_norm_g):
    f = lambda a: np.ascontiguousarray(np.asarray(a, dtype=np.float32))
    x, c, ctx, c_ctx = f(x), f(c), f(ctx), f(c_ctx)
    Bsz, SEQ, _ = x.shape
    NT = SEQ // 128
    NT_OWN = NT // 2
    NT_OTH = NT - NT_OWN
    NCTX = ctx.shape[1] // 128
    rows_total = SEQ // GRID_W
    n_cores = 2 * Bsz
    key = (NT_OWN, NT_OTH, NCTX)
    if key not in _PROG_CACHE:
        _PROG_CACHE[key] = build_program(*key)
    nc = _PROG_CACHE[key]

    w_ada0, b_ada0 = f(w_ada)[0], f(b_ada)[0]
    w_in0 = f(w_in)[0]
    w_dec0, b_dec0 = f(w_decay)[0], f(b_decay)[0]
    tri_f = np.triu(np.ones((128, 128), np.float32))
    cmat = np.stack([np.eye(128, dtype=np.float32), tri_f, np.ascontiguousarray(tri_f.T)])
    common = {
        "w_ada": w_ada0,
        "b_adaT": np.ascontiguousarray(b_ada0.reshape(48, 128).T),
        "b_gtB": np.ascontiguousarray(np.broadcast_to(
            np.stack([b_ada0[2 * D:3 * D], b_ada0[5 * D:6 * D]])[None], (128, 2, D))),
        "g12T": np.ascontiguousarray(np.stack([f(norm1_g)[0].reshape(KC, 128).T, f(norm2_g)[0].reshape(KC, 128).T], axis=1)),
        "ggB": np.ascontiguousarray(np.broadcast_to(f(gla_norm_g)[0][None, :], (128, 128))),
        "w_pool": f(w_pool)[0],
        "pscT": np.ascontiguousarray(f(pool_scale)[0].reshape(4, 128).T),
        "w_out": f(w_out)[0],
        "w_r": np.ascontiguousarray(np.concatenate([f(w_router_group)[0], f(w_router_expert)[0]], axis=1)),
        "w_ei": f(w_expert_in)[0],
        "w_eo": f(w_expert_out)[0],
        "fgB": np.ascontiguousarray(np.broadcast_to(f(final_norm_g)[None, :], (128, D))),
        "cmat": cmat,
    }
    per_par = []
    for par in range(2):
        d0, d1 = (0, 1) if par == 0 else (1, 0)
        wi = w_in0.copy()
        if par:
            wi[:, OFF_A:OFF_A + 16] = w_in0[:, OFF_A + 16:OFF_A + 32]
            wi[:, OFF_A + 16:OFF_A + 32] = w_in0[:, OFF_A:OFF_A + 16]
        wdec = np.zeros((33, 512), np.float32)
        wdec[0:16, 0:256] = w_dec0[d0]
        wdec[16:32, 256:512] = w_dec0[d1]
        wdec[32, 0:256] = b_dec0[d0]
        wdec[32, 256:512] = b_dec0[d1]
        per_par.append({"w_in": wi, "wdec": wdec, "apool": _pool_blocks(rows_total, par)})
    in_maps = []
    for core in range(n_cores):
        b, par = core // 2, core % 2
        m = dict(common)
        m.update(per_par[par])
        xb, cb = x[b], ctx[b]
        if par:
            xb, cb = xb[::-1], cb[::-1]
        m["xs"] = np.ascontiguousarray(xb)
        m["ctxs"] = np.ascontiguousarray(cb)
        m["cvec"] = np.ascontiguousarray(np.stack([c[b].reshape(KC, 128).T, c_ctx.reshape(KC, 128).T], axis=2))
        in_maps.append(m)
    res = run_bass_kernel_spmd(nc, in_maps, core_ids=list(range(n_cores)))
    out = np.empty((Bsz, SEQ, D), np.float32)
    half = NT_OWN * 128
    for core in range(n_cores):
        b, par = core // 2, core % 2
        yv = res.results[core]["y"]
        if par == 0:
            out[b, :half] = yv
        else:
            out[b, half:] = yv[::-1]
    return out
```

```python
import numpy as np
import concourse.bass as bass
import concourse.mybir as mybir
from concourse.bass_utils import run_bass_kernel_spmd

F32 = mybir.dt.float32
BF16 = mybir.dt.bfloat16
AF = mybir.ActivationFunctionType
ALU = mybir.AluOpType
AX = mybir.AxisListType


class Buf:
    __slots__ = ("name", "last_w", "readers")

    def __init__(self, name):
        self.name = name
        self.last_w = None
        self.readers = []


class DSem:
    __slots__ = ("sem", "count", "open_ops")

    def __init__(self, sem):
        self.sem = sem
        self.count = 0
        self.open_ops = []


class Op:
    __slots__ = ("eng", "fn", "deps", "signal", "semval", "dsem", "is_dma", "idx")


class Prog:
    ENGS = ("pe", "act", "dve", "pool", "sp")

    def __init__(self, nc):
        self.nc = nc
        self.ops = []
        self.by_eng = {e: [] for e in self.ENGS}

    def op(self, eng, fn, reads=(), writes=(), dsem=None, batch=False):
        o = Op()
        o.eng = eng
        o.fn = fn
        o.deps = []
        o.signal = False
        o.semval = None
        o.dsem = dsem
        o.is_dma = dsem is not None
        o.idx = len(self.ops)
        if o.is_dma:
            dsem.count += 16
            o.semval = dsem.count
            o.signal = True
            if batch:
                for q in dsem.open_ops:
                    q.semval = dsem.count
            else:
                dsem.open_ops = []
            dsem.open_ops.append(o)
        deps = set()
        for b in reads:
            if b.last_w is not None:
                deps.add(b.last_w)
        for b in writes:
            if b.last_w is not None:
                deps.add(b.last_w)
            for r in b.readers:
                deps.add(r)
        same_batch = set(id(q) for q in dsem.open_ops) if (o.is_dma and batch) else ()
        for p in deps:
            if p is o or id(p) in same_batch:
                continue
            if p.idx < getattr(self, "n_emitted_ops", 0):
                continue
            if (not p.is_dma) and (not o.is_dma) and p.eng == eng:
                if eng == "pe":
                    continue
                raw = any(b.last_w is p for b in reads) or any(b.last_w is p for b in writes)
                if not raw:
                    continue
            o.deps.append(p)
            p.signal = True
        for b in reads:
            b.readers.append(o)
        for b in writes:
            b.last_w = o
            b.readers = []
        self.ops.append(o)
        self.by_eng[eng].append(o)
        return o

    def emit(self, block, sems, final=()):
        self.sems = sems
        self.bar = None
        self.emit_phase(block, final=final)

    def emit_phase(self, block, final=()):
        sems = self.sems
        if not hasattr(self, "cnt"):
            self.cnt = {e: 0 for e in self.ENGS}
            self.waited = {e: {} for e in self.ENGS}
            self.emitted = {e: 0 for e in self.ENGS}
            self.phase = 0
        self.phase += 1
        self.n_emitted_ops = len(self.ops)
        streams = {}
        for e in self.ENGS:
            streams[e] = self.by_eng[e][self.emitted[e]:]
            self.emitted[e] = len(self.by_eng[e])
            c = self.cnt[e]
            for o in streams[e]:
                if (not o.is_dma) and o.signal:
                    c += 1
                    o.semval = c
            self.cnt[e] = c
        prog = self
        phase = self.phase

        def run(engname, engine):
            waited = prog.waited[engname]
            my_dsems = {}
            for o in streams[engname]:
                for p in o.deps:
                    if p.is_dma:
                        s, v = p.dsem.sem, p.semval
                    else:
                        s, v = sems[p.eng], p.semval
                    key = s.num
                    if waited.get(key, 0) >= v:
                        continue
                    waited[key] = v
                    engine.wait_ge(s, v)
                ins = o.fn(engine)
                if o.is_dma:
                    ins.then_inc(o.dsem.sem, 16)
                    my_dsems[o.dsem.sem.num] = (o.dsem.sem, o.semval)
                elif o.signal:
                    ins.then_inc(sems[engname], 1)
            for s, v in my_dsems.values():
                if waited.get(s.num, 0) < v:
                    waited[s.num] = v
                    engine.wait_ge(s, v)
            if engname == "sp":
                for d in final:
                    engine.wait_ge(d.sem, d.count)
            if prog.bar is not None:
                engine.sem_inc(prog.bar, 1)
                engine.wait_ge(prog.bar, 5 * phase)

        @block.tensor
        def _(eng):
            run("pe", eng)

        @block.scalar
        def _(eng):
            run("act", eng)

        @block.vector
        def _(eng):
            run("dve", eng)

        @block.gpsimd
        def _(eng):
            run("pool", eng)

        @block.sync
        def _(eng):
            run("sp", eng)

    def mm(self, out, lhsT, rhs, start=True, stop=True, reads=(), writes=()):
        return self.op("pe", lambda e: e.matmul(out, lhsT, rhs, start=start, stop=stop), reads, writes)

    def tr(self, out, in_, ident, reads=(), writes=()):
        return self.op("pe", lambda e: e.transpose(out, in_, ident), reads, writes)

    def act(self, out, in_, func, reads=(), writes=(), **kw):
        return self.op("act", lambda e: e.activation(out, in_, func, **kw), reads, writes)

    def tt(self, eng, out, in0, in1, op, reads=(), writes=()):
        return self.op(eng, lambda e: e.tensor_tensor(out, in0, in1, op), reads, writes)

    def ts(self, eng, out, in0, s1, s2, op0, op1=None, reads=(), writes=()):
        if op1 is None:
            return self.op(eng, lambda e: e.tensor_scalar(out, in0, s1, s2, op0), reads, writes)
        return self.op(eng, lambda e: e.tensor_scalar(out, in0, s1, s2, op0, op1), reads, writes)

    def stt(self, eng, out, in0, scalar, in1, op0, op1, reads=(), writes=()):
        return self.op(eng, lambda e: e.scalar_tensor_tensor(out, in0, scalar, in1, op0, op1), reads, writes)

    def cp(self, eng, out, in_, reads=(), writes=()):
        if eng == "act":
            return self.op(eng, lambda e: e.copy(out, in_), reads, writes)
        return self.op(eng, lambda e: e.tensor_copy(out, in_), reads, writes)

    def dma(self, eng, out, in_, dsem, reads=(), writes=(), batch=False):
        return self.op(eng, lambda e: e.dma_start(out=out, in_=in_), reads, writes, dsem=dsem, batch=batch)

    def reset_tracking(self, bufs):
        for b in bufs:
            b.last_w = None
            b.readers = []


class TB:
    __slots__ = ("t", "b")

    def __init__(self, t, name):
        self.t = t
        self.b = Buf(name)


class Ring:
    def __init__(self, items):
        self.items = items
        self.i = -1

    def next(self):
        self.i = (self.i + 1) % len(self.items)
        return self.items[self.i]

    def cur(self):
        return self.items[self.i]


D = 1024
KC = 8
GRID_W = 64
IN_COLS = 2080
OFF_Q, OFF_K, OFF_V, OFF_G, OFF_A, OFF_P = 0, 256, 512, 1024, 1536, 1568
NEXP = 32
DEXP = 256
EPS = 1e-6
POOL_W = (2, 4, 8, 16)
POOL_R = (1, 1, 2, 4)
HALO = 4
BIG = 1.0e30


def pool_block_table():
    tab = {}
    n = 0
    for gi, R in enumerate(POOL_R):
        for v in range(R + 1):
            for dt in range(-R, R + 1):
                if v < R and v + dt < 0:
                    continue
                tab[(v, gi, dt)] = n
                n += 1
    return tab, n


POOL_TAB, NBLK_A = pool_block_table()


_STOP = 0


def build_program(NT_OWN, NT_OTH, NCTX):
    from contextlib import ExitStack
    NT_ALL = NT_OWN + NT_OTH
    NTOK = NT_OWN * 128
    TPB = min(16, NT_OWN)
    NBLK = NT_OWN // TPB
    nc = bass.Bass("TRN2", target_bir_lowering=False)

    def din(name, shape, dt=F32):
        return nc.dram_tensor(name, list(shape), dt, kind="ExternalInput").ap()

    xs = din("xs", [NT_ALL * 128, D])
    ctxs = din("ctxs", [NCTX * 128, D])
    cvec = din("cvec", [128, KC, 2])
    w_ada = din("w_ada", [D, 6 * D])
    b_adaT = din("b_adaT", [128, 48])
    b_gtB = din("b_gtB", [128, 2, D])
    g12T = din("g12T", [128, 2, KC])
    w_in = din("w_in", [D, IN_COLS])
    wdec = din("wdec", [33, 512])
    ggB_d = din("ggB", [128, 128])
    w_pool = din("w_pool", [4, 128, 128])
    pscT = din("pscT", [128, 4])
    w_out = din("w_out", [D, D])
    w_r = din("w_r", [D, 36])
    w_ei = din("w_ei", [NEXP, D, 2 * DEXP])
    w_eo = din("w_eo", [NEXP, DEXP, D])
    fgB_d = din("fgB", [128, D])
    cmat = din("cmat", [3, 128, 128])
    apool = din("apool", [NBLK_A, 128, 128])
    y = nc.dram_tensor("y", [NTOK, D], F32, kind="ExternalOutput").ap()
    hT_scr = nc.dram_tensor("hT_scr", [NT_OWN, 128, KC, 128], BF16).ap()
    pl_scr = nc.dram_tensor("pl_scr", [NT_OWN, 128, 4, 128], BF16).ap()
    S_scr = nc.dram_tensor("S_scr", [NT_OWN, 128, 2, 128], BF16).ap()
    h2_scr = nc.dram_tensor("h2_scr", [128, KC, NTOK], BF16).ap()

    ges = ExitStack()
    with ges:
        def gsb(name, shape, dt):
            return TB(ges.enter_context(nc.sbuf_tensor("sg_" + name, list(shape), dt)), name)

        def gsem(name):
            return ges.enter_context(nc.semaphore(name))

        P = Prog(nc)
        P.sems = {e: gsem("s_" + e) for e in Prog.ENGS}
        P.bar = gsem("bar")
        dsem_pool = [DSem(gsem("d%d" % i)) for i in range(70)]
        dsem_i = [0]

        def new_dsem():
            d = dsem_pool[dsem_i[0]]
            dsem_i[0] += 1
            return d

        hT_b = [Buf("hTs%d" % i) for i in range(NT_OWN)]
        pl_b = [Buf("pls%d" % i) for i in range(NT_OWN)]
        S_b = [Buf("Ss%d" % i) for i in range(NT_OWN)]
        h2_b = [Buf("h2s%d" % i) for i in range(NT_OWN)]
        y_b = [Buf("ys%d" % i) for i in range(NT_OWN)]

        ident = gsb("ident", [128, 128], BF16)
        triF = gsb("triF", [128, 128], BF16)
        triB = gsb("triB", [128, 128], BF16)
        tri = (triF, triB)
        modv = gsb("modv", [128, 6, KC], F32)
        gtB = gsb("gtB", [128, 2, D], F32)
        fgB = gsb("fgB", [128, D], F32)
        ggB = gsb("ggB", [128, 128], F32)
        psc = gsb("psc", [128, 4], F32)
        gates = gsb("gates", [128, NT_OWN, NEXP], F32)
        dconst = new_dsem()
        dconst_p = new_dsem()
        for tbv, src in ((ident, cmat[0]), (triF, cmat[1]), (triB, cmat[2])):
            P.dma("pool", tbv.t[:], src, dconst_p, writes=[tbv.b], batch=True)
        P.dma("sp", fgB.t[:], fgB_d, dconst, writes=[fgB.b], batch=True)
        P.dma("sp", ggB.t[:], ggB_d, dconst, writes=[ggB.b], batch=True)
        P.dma("sp", psc.t[:], pscT, dconst, writes=[psc.b], batch=True)

        wes = ExitStack()
        w_in_sb = TB(wes.enter_context(nc.sbuf_tensor("sg_w_in_sb", [128, KC, IN_COLS], BF16)), "w_in_sb")
        wdec_sb = TB(wes.enter_context(nc.sbuf_tensor("sg_wdec_sb", [33, 512], BF16)), "wdec_sb")
        dw = new_dsem()
        for kc in range(KC):
            P.dma("pool", w_in_sb.t[:, kc, :], w_in[kc * 128:(kc + 1) * 128, :], dw, writes=[w_in_sb.b], batch=True)
        P.dma("pool", wdec_sb.t[:], wdec, dw, writes=[wdec_sb.b], batch=True)

        with ExitStack() as es:
            def sb(name, shape, dt):
                return TB(es.enter_context(nc.sbuf_tensor("sb_" + name, list(shape), dt)), name)

            def ps(name, shape, dt=F32):
                return TB(es.enter_context(nc.psum_tensor("ps_" + name, list(shape), dt)), name)

            cv = sb("cv", [128, KC, 2], F32)
            sv = sb("sv", [128, KC, 2], F32)
            srep = sb("srep", [128, KC, 128], F32)
            wst = Ring([sb("wst%d" % i, [128, KC, 512], F32) for i in range(3)])
            wst_d = [new_dsem(), new_dsem(), new_dsem()]
            wst_q = ("sp", "act", "sp")
            bT = sb("bT", [128, 48], F32)
            bgt = sb("bgt", [128, 2, D], F32)
            g12 = sb("g12", [128, 2, KC], F32)
            mv = sb("mv", [128, 4, KC, 2], F32)
            psg = Ring([ps("psg%d" % i, [128, 512]) for i in range(2)])
            psv = Ring([ps("psv%d" % i, [128, 8]) for i in range(2)])
            P.dma("sp", cv.t[:], cvec, dconst, writes=[cv.b], batch=True)
            P.dma("sp", bT.t[:], b_adaT, dconst, writes=[bT.b], batch=True)
            P.dma("sp", bgt.t[:], b_gtB, dconst, writes=[bgt.b], batch=True)
            P.dma("sp", g12.t[:], g12T, dconst, writes=[g12.b], batch=True)
            P.act(sv.t[:], cv.t[:], AF.Silu, reads=[cv.b], writes=[sv.b])
            P.cp("dve", srep.t[:], sv.t[:, :, 0:1].to_broadcast([128, KC, 128]), reads=[sv.b], writes=[srep.b])
            vec_of_block = {0: 0, 1: 0, 2: 1, 3: 1, 6: 2, 7: 2, 8: 3, 9: 3}
            for j in range(12):
                w = wst.next()
                P.dma(wst_q[wst.i], w.t[:], w_ada[:, j * 512:(j + 1) * 512].rearrange("(k p) c -> p k c", p=128),
                      wst_d[wst.i], writes=[w.b])
                half = j % 2
                if j in (4, 5, 10, 11):
                    gi = 0 if j < 6 else 1
                    pt = psg.next()
                    for kc in range(KC):
                        P.mm(pt.t[:, :], srep.t[:, kc, :], w.t[:, kc, :], start=(kc == 0), stop=(kc == KC - 1),
                             reads=[srep.b, w.b], writes=[pt.b])
                    P.tt("dve", gtB.t[:, gi, half * 512:(half + 1) * 512], pt.t[:, :],
                         bgt.t[:, gi, half * 512:(half + 1) * 512], ALU.add, reads=[pt.b, bgt.b], writes=[gtB.b])
                else:
                    vi = vec_of_block[j]
                    pt = psv.next()
                    for cc in range(4):
                        for kc in range(KC):
                            P.mm(pt.t[:, cc * 2:cc * 2 + 2], w.t[:, kc, cc * 128:(cc + 1) * 128], sv.t[:, kc, :],
                                 start=(kc == 0), stop=(kc == KC - 1), reads=[w.b, sv.b], writes=[pt.b])
                    P.tt("dve", mv.t[:, vi, half * 4:half * 4 + 4, :],
                         pt.t[:, :].rearrange("p (a b) -> p a b", b=2),
                         bT.t[:, j * 4:j * 4 + 4].unsqueeze(2).to_broadcast([128, 4, 2]), ALU.add,
                         reads=[pt.b, bT.b], writes=[mv.b])
            P.stt("dve", modv.t[:, 0, :], mv.t[:, 1, :, 0], 1.0, g12.t[:, 0, :], ALU.add, ALU.mult,
                  reads=[mv.b, g12.b], writes=[modv.b])
            P.cp("dve", modv.t[:, 1, :], mv.t[:, 0, :, 0], reads=[mv.b], writes=[modv.b])
            P.stt("dve", modv.t[:, 2, :], mv.t[:, 1, :, 1], 1.0, g12.t[:, 0, :], ALU.add, ALU.mult,
                  reads=[mv.b, g12.b], writes=[modv.b])
            P.cp("dve", modv.t[:, 3, :], mv.t[:, 0, :, 1], reads=[mv.b], writes=[modv.b])
            P.stt("dve", modv.t[:, 4, :], mv.t[:, 3, :, 0], 1.0, g12.t[:, 1, :], ALU.add, ALU.mult,
                  reads=[mv.b, g12.b], writes=[modv.b])
            P.cp("dve", modv.t[:, 5, :], mv.t[:, 2, :, 0], reads=[mv.b], writes=[modv.b])
            with nc.Block() as block:
                P.emit_phase(block)
        if _STOP == 1:
            return nc

        def run_pipeline(tiles, order):
            K = len(order)
            N = len(tiles)
            for i in range(N + K - 1):
                for s in order:
                    t = i - s
                    if 0 <= t < N and tiles[t][s] is not None:
                        tiles[t][s]()

        with ExitStack() as es:
            def sb(name, shape, dt):
                return TB(es.enter_context(nc.sbuf_tensor("sb_" + name, list(shape), dt)), name)

            def ps(name, shape, dt=F32):
                return TB(es.enter_context(nc.psum_tensor("ps_" + name, list(shape), dt)), name)

            def ring(name, n, shape, dt):
                return Ring([sb("%s%d" % (name, i), shape, dt) for i in range(n)])

            def dring(n):
                return [new_dsem() for _ in range(n)]


            xt_r = ring("xt", 3, [128, D], F32)
            xt_d = dring(3)
            sqj = sb("sqj", [128, D], BF16)
            stat_r = ring("stat", 4, [128, 4], F32)
            xn_r = ring("xn", 2, [128, D], BF16)
            mtx_r = ring("mtx", 2, [128, D], F32)
            hT_r = ring("hT", 3, [128, KC, 128], BF16)
            hT_d = dring(3)
            hTs_d = dring(3)
            qk_r = ring("qk", 4, [128, 512], F32)
            v_r = ring("v_sb", 7, [128, 512], BF16)
            a_r = ring("a_sb", 3, [33, 128], BF16)
            e1_r = ring("e1", 2, [128, 512], F32)
            l_r = ring("l_bf", 2, [128, 512], BF16)
            Ek_r = ring("Ek", 3, [128, 512], F32)
            el_r = ring("elast", 6, [128, 4], F32)
            kst_r = ring("kst", 2, [128, 2, 128], BF16)
            kstm_r = ring("kstm", 3, [128, 2, 128], BF16)
            Sb = sb("Sb", [128, 2, 128], F32)
            Sf = sb("Sf", [128, 2, 128], F32)
            Sbbf_r = ring("Sbbf", 5, [128, 2, 128], BF16)
            Sbbf_d = dring(5)
            Sbbfs_d = dring(5)
            Sfbf_r = ring("Sfbf", 3, [128, 2, 128], BF16)
            B0 = ps("B0", [128, 1024], BF16)
            B1 = ps("B1", [128, 512])
            B2 = ps("B2", [128, 512])
            B3 = ps("B3", [128, 512])
            B4 = ps("B4", [128, 512])
            B5 = ps("B5", [128, 512])
            B6 = ps("B6", [128, 512])
            B7a = ps("B7a", [128, 512])
            B7b = B0

            for r_ in a_r.items:
                P.op("dve", lambda e, r_=r_: e.memset(r_.t[:], 1.0), writes=[r_.b])
            P.op("dve", lambda e: e.memset(Sb.t[:], 0.0), writes=[Sb.b])
            P.op("dve", lambda e: e.memset(Sf.t[:], 0.0), writes=[Sf.b])
            for r_ in Sfbf_r.items + Sbbf_r.items:
                P.op("dve", lambda e, r_=r_: e.memset(r_.t[:], 0.0), writes=[r_.b])

            class CX:
                pass

            def load_x(src_ap):
                xt = xt_r.next()
                P.dma("sp", xt.t[:], src_ap, xt_d[xt_r.i], writes=[xt.b])
                return xt

            def norm_act(xt):
                st = stat_r.next()
                P.act(sqj.t[:], xt.t[:], AF.Square, accum_out=st.t[:, 0:1], reads=[xt.b], writes=[st.b])
                P.act(st.t[:, 1:2], st.t[:, 0:1], AF.Ln, bias=EPS, scale=1.0 / D, reads=[st.b], writes=[st.b])
                P.act(st.t[:, 2:3], st.t[:, 1:2], AF.Exp, scale=-0.5, reads=[st.b], writes=[st.b])
                return st

            def norm_dve(xt, st, on_act=False):
                xn = xn_r.next()
                if on_act:
                    P.act(xn.t[:], xt.t[:], AF.Copy, scale=st.t[:, 2:3], reads=[xt.b, st.b], writes=[xn.b])
                else:
                    P.ts("dve", xn.t[:], xt.t[:], st.t[:, 2:3], None, ALU.mult, reads=[xt.b, st.b], writes=[xn.b])
                return xn

            def transpose_mod(xn, gi, hT):
                for kc in range(KC):
                    P.tr(B0.t[:, kc * 128:(kc + 1) * 128], xn.t[:, kc * 128:(kc + 1) * 128], ident.t[:],
                         reads=[xn.b, ident.b], writes=[B0.b])
                mx = mtx_r.next()
                P.tt("dve", mx.t[:].rearrange("p (k t) -> p k t", k=KC), B0.t[:, :].rearrange("p (k t) -> p k t", k=KC),
                     modv.t[:, gi, :].unsqueeze(2).to_broadcast([128, KC, 128]), ALU.mult,
                     reads=[B0.b, modv.b], writes=[mx.b])
                P.tt("pool", hT.t[:], mx.t[:].rearrange("p (k t) -> p k t", k=KC),
                     modv.t[:, gi + 1, :].unsqueeze(2).to_broadcast([128, KC, 128]), ALU.add,
                     reads=[mx.b, modv.b], writes=[hT.b])

            def proj_fm(hT, col0, ncol, out_ap, out_b):
                for kc in range(KC):
                    P.mm(out_ap, w_in_sb.t[:, kc, col0:col0 + ncol], hT.t[:, kc, :], start=(kc == 0),
                         stop=(kc == KC - 1), reads=[w_in_sb.b, hT.b], writes=[out_b])

            def proj_tm(hT, col0, out_ap, out_b):
                for kc in range(KC):
                    P.mm(out_ap, hT.t[:, kc, :], w_in_sb.t[:, kc, col0:col0 + 512], start=(kc == 0),
                         stop=(kc == KC - 1), reads=[w_in_sb.b, hT.b], writes=[out_b])

            def decay_mm(a, dirs, PB):
                P.mm(PB.t[:, :], a.t[0:33, :], wdec_sb.t[0:33, :], reads=[a.b, wdec_sb.b], writes=[PB.b])
                e1 = e1_r.next()
                lo, hi = (0, 512) if len(dirs) == 2 else (dirs[0] * 256, dirs[0] * 256 + 256)
                P.act(e1.t[:, lo:hi], PB.t[:, lo:hi], AF.Exp, scale=-1.0, reads=[PB.b], writes=[e1.b])
                l = l_r.next()
                P.act(l.t[:, lo:hi], e1.t[:, lo:hi], AF.Ln, bias=1.0, reads=[e1.b], writes=[l.b])
                return l, lo, hi

            def cumsum_mm(l, dirs):
                for d in dirs:
                    for c in range(2):
                        j = d * 2 + c
                        P.mm(B3.t[:, j * 128:(j + 1) * 128], l.t[:, j * 128:(j + 1) * 128], tri[d].t[:],
                             reads=[l.b, tri[d].b], writes=[B3.b])

            def kst_make(d, Ek, ksrc, k0):
                el = el_r.next()
                last = 127 if d == 0 else 0
                cs3 = B3.t[:, :].rearrange("p (j t) -> p j t", t=128)
                P.act(el.t[:, 0:2], cs3[:, 2 * d:2 * d + 2, last], AF.Exp, scale=-1.0 / 16, reads=[B3.b], writes=[el.b])
                kst = kst_r.next()
                for c in range(2):
                    P.stt("dve", kst.t[:, c, :], ksrc.t[:, k0 + c * 128:k0 + (c + 1) * 128], el.t[:, c:c + 1],
                          Ek.t[:, (d * 2 + c) * 128:(d * 2 + c + 1) * 128], ALU.mult, ALU.mult,
                          reads=[ksrc.b, el.b, Ek.b], writes=[kst.b])
                return el, kst

            def kst_transpose(kst, PB):
                for c in range(2):
                    P.tr(PB.t[:, c * 128:(c + 1) * 128], kst.t[:, c, :], ident.t[:], reads=[kst.b, ident.b],
                         writes=[PB.b])
                kstm = kstm_r.next()
                P.cp("act", kstm.t[:].rearrange("p c t -> p (c t)"), PB.t[:, 0:256], reads=[PB.b], writes=[kstm.b])
                return kstm

            def state_apply(el, kstm, v, S, Sbf_ring, PB):
                for c in range(2):
                    P.mm(PB.t[:, c * 256:(c + 1) * 256], kstm.t[:, c, :], v.t[:, c * 256:(c + 1) * 256],
                         reads=[kstm.b, v.b], writes=[PB.b])
                for c in range(2):
                    for hf in range(2):
                        r0 = hf * 64
                        P.stt("dve", S.t[r0:r0 + 64, c, :], S.t[r0:r0 + 64, c, :], el.t[r0:r0 + 64, c:c + 1],
                              PB.t[r0:r0 + 64, c * 256 + hf * 128:c * 256 + (hf + 1) * 128], ALU.mult, ALU.add,
                              reads=[S.b, el.b, PB.b], writes=[S.b])
                sbf = Sbf_ring.next()
                P.cp("pool", sbf.t[:], S.t[:], reads=[S.b], writes=[sbf.b])
                return sbf

            with ExitStack() as esa:
                def sba(name, shape, dt):
                    return TB(esa.enter_context(nc.sbuf_tensor("sa_" + name, list(shape), dt)), name)

                NP = 14
                p_r = [sba("p_sb%d" % i, [128, 512], BF16) for i in range(NP)]
                diff_r = Ring([sba("diff%d" % i, [128, 4, 128], BF16) for i in range(2)])
                pl_r = Ring([sba("plbf%d" % i, [128, 4, 128], BF16) for i in range(2)])
                pl_d = dring(2)
                wpool_sb = sba("wpool_sb", [128, 4, 128], BF16)
                A_sb = sba("A_sb", [128, NBLK_A, 128], BF16)
                dw2 = new_dsem()
                P.dma("pool", wpool_sb.t[:], w_pool.rearrange("g c d -> c g d"), dw2, writes=[wpool_sb.b], batch=True)
                for b0 in range(0, NBLK_A, 4):
                    b1 = min(NBLK_A, b0 + 4)
                    P.dma("pool", A_sb.t[:, b0:b1, :], apool[b0:b1].rearrange("n k t -> k n t"), dw2, writes=[A_sb.b],
                          batch=True)

                def slot_of(tile):
                    return tile % NP

                def pool_fin1(tt):
                    for gi, R in enumerate(POOL_R):
                        v_ = tt if tt < R else R
                        dts = [dt for dt in range(-R, R + 1) if tt + dt >= 0]
                        for n_, dt in enumerate(dts):
                            pt = p_r[slot_of(tt + dt)]
                            blk = POOL_TAB[(v_, gi, dt)]
                            P.mm(B2.t[:, gi * 128:(gi + 1) * 128], pt.t[:, gi * 128:(gi + 1) * 128], A_sb.t[:, blk, :],
                                 start=(n_ == 0), stop=(n_ == len(dts) - 1), reads=[pt.b, A_sb.b], writes=[B2.b])
                    df = diff_r.next()
                    P.cp("act", df.t[:].rearrange("p g t -> p (g t)"), B2.t[:, :], reads=[B2.b], writes=[df.b])
                    return df

                def pool_fin2(tt, df):
                    for gi in range(4):
                        P.mm(B6.t[:, gi * 128:(gi + 1) * 128], wpool_sb.t[:, gi, :], df.t[:, gi, :],
                             reads=[wpool_sb.b, df.b], writes=[B6.b])
                    pl = pl_r.next()
                    P.tt("dve", pl.t[:], B6.t[:, :].rearrange("p (g t) -> p g t", g=4),
                         psc.t[:, :].unsqueeze(2).to_broadcast([128, 4, 128]), ALU.mult, reads=[B6.b, psc.b], writes=[pl.b])
                    P.dma("pool", pl_scr[tt], pl.t[:], pl_d[pl_r.i], reads=[pl.b], writes=[pl_b[tt]])

                def a_tile(src_ap, gi, d, S, Sring, own_tau, pool_tau, fin_tt):
                    cx = CX()

                    def s0():
                        cx.xt = load_x(src_ap)
                        cx.st = norm_act(cx.xt)

                    def s1():
                        cx.xn = norm_dve(cx.xt, cx.st)

                    def s2():
                        cx.hT = hT_r.next()
                        transpose_mod(cx.xn, gi, cx.hT)
                        if own_tau is not None:
                            P.dma("pool", hT_scr[own_tau], cx.hT.t[:], hTs_d[hT_r.i], reads=[cx.hT.b],
                                  writes=[hT_b[own_tau]])

                    def s3():
                        hT = cx.hT
                        proj_fm(hT, OFF_K, 128, B1.t[:, 0:128], B1.b)
                        proj_fm(hT, OFF_K + 128, 128, B1.t[:, 128:256], B1.b)
                        proj_tm(hT, OFF_V, B2.t[:, :], B2.b)
                        proj_fm(hT, OFF_A, 32, B7a.t[0:32, 0:128], B7a.b)
                        cx.kq = qk_r.next()
                        P.cp("dve", cx.kq.t[:, 0:256], B1.t[:, 0:256], reads=[B1.b], writes=[cx.kq.b])
                        cx.v = v_r.next()
                        P.cp("act", cx.v.t[:], B2.t[:, :], reads=[B2.b], writes=[cx.v.b])
                        cx.a = a_r.next()
                        P.cp("act", cx.a.t[0:32, :], B7a.t[0:32, 0:128], reads=[B7a.b], writes=[cx.a.b])
                        if pool_tau is not None:
                            proj_tm(hT, OFF_P, B6.t[:, :], B6.b)
                            pt = p_r[slot_of(pool_tau)]
                            P.cp("dve", pt.t[:], B6.t[:, :], reads=[B6.b], writes=[pt.b])

                    def s4():
                        cx.l, cx.lo, cx.hi = decay_mm(cx.a, [d], B4)

                    def s5():
                        cumsum_mm(cx.l, [d])
                        Ek = cx.Ek = Ek_r.next()
                        P.act(Ek.t[:, cx.lo:cx.hi], B3.t[:, cx.lo:cx.hi], AF.Exp, scale=1.0 / 16, reads=[B3.b], writes=[Ek.b])
                        el = cx.el = el_r.next()
                        last = 127 if d == 0 else 0
                        cs3 = B3.t[:, :].rearrange("p (j t) -> p j t", t=128)
                        P.act(el.t[:, 0:2], cs3[:, 2 * d:2 * d + 2, last], AF.Exp, scale=-1.0 / 16, reads=[B3.b], writes=[el.b])

                    def s6():
                        kst = cx.kst = kst_r.next()
                        for c in range(2):
                            P.stt("dve", kst.t[:, c, :], cx.kq.t[:, c * 128:(c + 1) * 128], cx.el.t[:, c:c + 1],
                                  cx.Ek.t[:, (d * 2 + c) * 128:(d * 2 + c + 1) * 128], ALU.mult, ALU.mult,
                                  reads=[cx.kq.b, cx.el.b, cx.Ek.b], writes=[kst.b])

                    def s7():
                        cx.kstm = kst_transpose(cx.kst, B7b)

                    def s8():
                        if own_tau is not None:
                            sbf = Sring.cur()
                            P.dma("pool", S_scr[own_tau], sbf.t[:], Sbbfs_d[Sring.i], reads=[sbf.b], writes=[S_b[own_tau]])
                        state_apply(cx.el, cx.kstm, cx.v, S, Sring, B5)
                        if fin_tt is not None:
                            cx.df = pool_fin1(fin_tt)

                    def s9():
                        if fin_tt is not None:
                            pool_fin2(fin_tt, cx.df)

                    return [s0, s1, s2, s3, s4, s5, s6, s7, s8, s9]

                def fin_tile(tt):
                    cx = CX()

                    def s8():
                        cx.df = pool_fin1(tt)

                    def s9():
                        pool_fin2(tt, cx.df)

                    return [None] * 8 + [s8, s9]

                tilesA = []
                if NCTX == 1:
                    raise NotImplementedError
                for i in range(NCTX):
                    tilesA.append(a_tile(ctxs[i * 128:(i + 1) * 128, :], 2, 0, Sf, Sfbf_r, None, None, None))
                for i in reversed(range(NCTX)):
                    tilesA.append(a_tile(ctxs[i * 128:(i + 1) * 128, :], 2, 1, Sb, Sbbf_r, None, None, None))
                for tau in reversed(range(NT_ALL)):
                    tt = tau + HALO
                    tilesA.append(a_tile(xs[tau * 128:(tau + 1) * 128, :], 0, 1, Sb, Sbbf_r,
                                         tau if tau < NT_OWN else None,
                                         tau if tau < NT_OWN + HALO else None,
                                         tt if tt < NT_OWN else None))
                for tt in reversed(range(min(HALO, NT_OWN))):
                    tilesA.append(fin_tile(tt))
                run_pipeline(tilesA, [9, 8, 7, 2, 6, 5, 4, 3, 1, 0])
                with nc.Block() as block:
                    P.emit_phase(block)
            if _STOP == 4:
                return nc

            with ExitStack() as esb:
                def sbb(name, shape, dt):
                    return TB(esb.enter_context(nc.sbuf_tensor("sbb_" + name, list(shape), dt)), name)

                def ringb(name, n, shape, dt):
                    return Ring([sbb("%s%d" % (name, i), shape, dt) for i in range(n)])

                w_out_sb = sbb("w_out_sb", [128, KC, D], BF16)
                w_r_sb = sbb("w_r_sb", [128, KC, 36], BF16)
                dw3 = new_dsem()
                for kc in range(KC):
                    P.dma("pool", w_out_sb.t[:, kc, :], w_out[kc * 128:(kc + 1) * 128, :], dw3, writes=[w_out_sb.b], batch=True)
                P.dma("pool", w_r_sb.t[:], w_r.rearrange("(k p) c -> p k c", p=128), dw3, writes=[w_r_sb.b], batch=True)
                Eq_r = ringb("Eq", 2, [128, 512], F32)
                qb_r = ringb("qb", 3, [128, 2, 2, 2, 128], BF16)
                kb_r = ringb("kb", 2, [128, 4, 128], BF16)
                scm_r = ringb("scm", 2, [128, 8, 128], BF16)
                sg_r = ringb("sg", 6, [128, 512], BF16)
                sl_r = ringb("sl", 2, [128, 512], F32)
                ot_r = ringb("ot", 2, [128, 512], F32)
                mtm_r = ringb("mtm", 3, [128, 512], BF16)
                mixT_r = ringb("mixT", 3, [128, KC, 128], BF16)
                mixT_d = dring(3)
                mt_r = ringb("mt", 2, [128, D], F32)
                x1_r = ringb("x1", 3, [128, D], F32)
                x1_d = dring(3)
                h2_r = ringb("h2T", 3, [128, KC, 128], BF16)
                h2_d = dring(3)
                RG = 4
                assert NT_OWN % RG == 0
                rl_r = ringb("rl", 2, [128, RG, 36], F32)
                rw_r = ringb("rw", 2, [128, RG, 160], F32)
                rstate = {}
                for r_ in qb_r.items:
                    P.op("dve", lambda e, r_=r_: e.memset(r_.t[:], 0.0), writes=[r_.b])

                def b_tile(t):
                    cx = CX()

                    def s0():
                        cx.hT = hT_r.next()
                        P.dma("sp", cx.hT.t[:], hT_scr[t], hT_d[hT_r.i], reads=[hT_b[t]], writes=[cx.hT.b])

                    def s1():
                        hT = cx.hT
                        for c in range(4):
                            proj_fm(hT, c * 128, 128, B1.t[:, c * 128:(c + 1) * 128], B1.b)
                        proj_tm(hT, OFF_V, B2.t[:, :], B2.b)
                        proj_fm(hT, OFF_A, 32, B7a.t[0:32, 0:128], B7a.b)
                        proj_tm(hT, OFF_G, B6.t[:, :], B6.b)
                        cx.qk = qk_r.next()
                        P.cp("act", cx.qk.t[:], B1.t[:, :], reads=[B1.b], writes=[cx.qk.b])
                        cx.v = v_r.next()
                        P.cp("act", cx.v.t[:], B2.t[:, :], reads=[B2.b], writes=[cx.v.b])
                        cx.a = a_r.next()
                        P.cp("act", cx.a.t[0:32, :], B7a.t[0:32, 0:128], reads=[B7a.b], writes=[cx.a.b])
                        cx.sl = sl_r.next()
                        P.act(cx.sl.t[:], B6.t[:, :], AF.Silu, reads=[B6.b], writes=[cx.sl.b])

                    def s2():
                        cx.sg = sg_r.next()
                        P.tt("pool", cx.sg.t[:].rearrange("p (h e) -> p h e", h=4), cx.sl.t[:].rearrange("p (h e) -> p h e", h=4),
                             ggB.t[:, :].unsqueeze(1).to_broadcast([128, 4, 128]), ALU.mult, reads=[cx.sl.b, ggB.b],
                             writes=[cx.sg.b])
                        cx.l, _, _ = decay_mm(cx.a, [0, 1], B7a)

                    def s3():
                        cumsum_mm(cx.l, [0, 1])
                        Ek = cx.Ek = Ek_r.next()
                        Eq = cx.Eq = Eq_r.next()
                        P.act(Ek.t[:, :], B3.t[:, :], AF.Exp, scale=1.0 / 16, reads=[B3.b], writes=[Ek.b])
                        P.act(Eq.t[:, :], B3.t[:, :], AF.Exp, scale=-1.0 / 16, reads=[B3.b], writes=[Eq.b])
                        el = cx.el = el_r.next()
                        cs3 = B3.t[:, :].rearrange("p (j t) -> p j t", t=128)
                        P.act(el.t[:, 0:2], cs3[:, 0:2, 127], AF.Exp, scale=-1.0 / 16, reads=[B3.b], writes=[el.b])
                        cx.sbin = Sbbf_r.next()
                        P.dma("sp", cx.sbin.t[:], S_scr[t], Sbbf_d[Sbbf_r.i], reads=[S_b[t]], writes=[cx.sbin.b])

                    def s4():
                        qk, Ek, Eq, el = cx.qk, cx.Ek, cx.Eq, cx.el
                        kst = cx.kst = kst_r.next()
                        for c in range(2):
                            P.stt("dve", kst.t[:, c, :], qk.t[:, 256 + c * 128:256 + (c + 1) * 128], el.t[:, c:c + 1],
                                  Ek.t[:, c * 128:(c + 1) * 128], ALU.mult, ALU.mult,
                                  reads=[qk.b, el.b, Ek.b], writes=[kst.b])
                        qb = cx.qb = qb_r.next()
                        kb = cx.kb = kb_r.next()
                        for hp in range(2):
                            r0 = hp * 64
                            P.stt("dve", qb.t[r0:r0 + 64, :, :, hp, :],
                                  qk.t[r0:r0 + 64, 0:256].rearrange("p (c t) -> p c t", c=2).unsqueeze(1).to_broadcast([64, 2, 2, 128]),
                                  0.125, Eq.t[r0:r0 + 64, :].rearrange("p (d c t) -> p d c t", d=2, c=2), ALU.mult, ALU.mult,
                                  reads=[qk.b, Eq.b], writes=[qb.b])
                        P.tt("dve", kb.t[:].rearrange("p (d c) t -> p d (c t)", d=2),
                             qk.t[:, 256:512].unsqueeze(1).to_broadcast([128, 2, 256]),
                             Ek.t[:, :].rearrange("p (d x) -> p d x", d=2), ALU.mult, reads=[qk.b, Ek.b], writes=[kb.b])

                    def s5():
                        cx.kstm = kst_transpose(cx.kst, B0)
                        kb, qb = cx.kb, cx.qb
                        scb = (B4, B5)
                        for d in range(2):
                            for h in range(4):
                                c = h // 2
                                P.mm(scb[d].t[:, h * 128:(h + 1) * 128], kb.t[:, d * 2 + c, :],
                                     qb.t[:, d, c, h % 2, :], reads=[kb.b, qb.b], writes=[scb[d].b])
                        scm = cx.scm = scm_r.next()
                        for d in range(2):
                            P.tt("dve", scm.t[:, d * 4:(d + 1) * 4, :], scb[d].t[:, :].rearrange("p (h t) -> p h t", h=4),
                                 tri[d].t[:, :].unsqueeze(1).to_broadcast([128, 4, 128]), ALU.mult,
                                 reads=[scb[d].b, tri[d].b], writes=[scm.b])

                    def s6():
                        scm, v, qb, sbin = cx.scm, cx.v, cx.qb, cx.sbin
                        sfbf = Sfbf_r.cur()
                        for h in range(4):
                            c = h // 2
                            oo = B6.t[:, h * 128:(h + 1) * 128]
                            P.mm(oo, scm.t[:, h, :], v.t[:, h * 128:(h + 1) * 128], start=True, stop=False,
                                 reads=[scm.b, v.b], writes=[B6.b])
                            P.mm(oo, scm.t[:, 4 + h, :], v.t[:, h * 128:(h + 1) * 128], start=False, stop=False,
                                 reads=[scm.b, v.b], writes=[B6.b])
                            P.mm(oo, qb.t[:, 1, c, h % 2, :], sbin.t[:, c, :], start=False, stop=False,
                                 reads=[qb.b, sbin.b], writes=[B6.b])
                            P.mm(oo, qb.t[:, 0, c, h % 2, :], sfbf.t[:, c, :], start=False, stop=True,
                                 reads=[qb.b, sfbf.b], writes=[B6.b])
                        st = stat_r.next()
                        for h in range(4):
                            P.act(sqj.t[:, h * 128:(h + 1) * 128], B6.t[:, h * 128:(h + 1) * 128], AF.Square,
                                  accum_out=st.t[:, h:h + 1], reads=[B6.b], writes=[st.b])
                        P.act(st.t[:, 0:4], st.t[:, 0:4], AF.Ln, bias=EPS, scale=1.0 / 128, reads=[st.b], writes=[st.b])
                        P.act(st.t[:, 0:4], st.t[:, 0:4], AF.Exp, scale=-0.5, reads=[st.b], writes=[st.b])
                        ot = cx.ot = ot_r.next()
                        P.tt("dve", ot.t[:].rearrange("p (h e) -> p h e", h=4), B6.t[:, :].rearrange("p (h e) -> p h e", h=4),
                             st.t[:, 0:4].unsqueeze(2).to_broadcast([128, 4, 128]), ALU.mult, reads=[B6.b, st.b], writes=[ot.b])

                    def s7():
                        state_apply(cx.el, cx.kstm, cx.v, Sf, Sfbf_r, B3)
                        cx.mtm = mtm_r.next()
                        P.tt("pool", cx.mtm.t[:], cx.ot.t[:], cx.sg.t[:], ALU.mult, reads=[cx.ot.b, cx.sg.b], writes=[cx.mtm.b])
                        cx.mixT = mixT_r.next()
                        P.dma("sp", cx.mixT.t[:, 4:8, :], pl_scr[t], mixT_d[mixT_r.i], reads=[pl_b[t]], writes=[cx.mixT.b])

                    def s8():
                        mtm, mixT = cx.mtm, cx.mixT
                        for h in range(4):
                            P.tr(B0.t[:, h * 128:(h + 1) * 128], mtm.t[:, h * 128:(h + 1) * 128], ident.t[:],
                                 reads=[mtm.b, ident.b], writes=[B0.b])
                        P.cp("act", mixT.t[:, 0:4, :].rearrange("p k t -> p (k t)"), B0.t[:, 0:512], reads=[B0.b],
                             writes=[mixT.b])
                        cx.xt = load_x(xs[t * 128:(t + 1) * 128, :])

                    def s9():
                        mixT, xt = cx.mixT, cx.xt
                        ob = (B1, B2)
                        for half in range(2):
                            for kc in range(KC):
                                P.mm(ob[half].t[:, :], mixT.t[:, kc, :], w_out_sb.t[:, kc, half * 512:(half + 1) * 512],
                                     start=(kc == 0), stop=(kc == KC - 1), reads=[mixT.b, w_out_sb.b], writes=[ob[half].b])
                        x1 = cx.x1 = x1_r.next()
                        mt = mt_r.next()
                        for half in range(2):
                            P.tt("dve", mt.t[:, half * 512:(half + 1) * 512], ob[half].t[:, :],
                                 gtB.t[:, 0, half * 512:(half + 1) * 512], ALU.mult, reads=[ob[half].b, gtB.b], writes=[mt.b])
                        P.tt("pool", x1.t[:], mt.t[:], xt.t[:], ALU.add, reads=[mt.b, xt.b], writes=[x1.b])
                        P.dma("pool", y[t * 128:(t + 1) * 128, :], x1.t[:], x1_d[x1_r.i], reads=[x1.b], writes=[y_b[t]])

                    def s10():
                        cx.st = norm_act(cx.x1)

                    def s10b():
                        cx.xn = norm_dve(cx.x1, cx.st, on_act=True)

                    def s11():
                        cx.h2 = h2_r.next()
                        transpose_mod(cx.xn, 4, cx.h2)
                        P.dma("pool", h2_scr[:, :, t * 128:(t + 1) * 128], cx.h2.t[:], h2_d[h2_r.i], reads=[cx.h2.b],
                              writes=[h2_b[t]])

                    def s12():
                        h2 = cx.h2
                        for kc in range(KC):
                            P.mm(B7a.t[:, 128:164], h2.t[:, kc, :], w_r_sb.t[:, kc, :], start=(kc == 0), stop=(kc == KC - 1),
                                 reads=[h2.b, w_r_sb.b], writes=[B7a.b])
                        if t % RG == 0:
                            rstate["L"] = rl_r.next()
                        L = rstate["L"]
                        cx.L = L
                        P.cp("dve", L.t[:, t % RG, :], B7a.t[:, 128:164], reads=[B7a.b], writes=[L.b])

                    def r13():
                        L = cx.L
                        Wt = cx.W = rw_r.next()
                        W = Wt.t
                        rb = [Wt.b]
                        lg = L.t[:, :, 0:4]
                        gmax_b = W[:, :, 4:5].to_broadcast([128, RG, 4])
                        P.op("dve", lambda e: e.tensor_reduce(W[:, :, 4:5], lg, AX.X, ALU.max), reads=[L.b], writes=rb)
                        P.tt("dve", W[:, :, 8:12], lg, gmax_b, ALU.is_equal, reads=[L.b] + rb, writes=rb)
                        P.tt("dve", W[:, :, 12:16], lg, gmax_b, ALU.subtract, reads=[L.b] + rb, writes=rb)
                        P.act(W[:, :, 16:20], W[:, :, 12:16], AF.Exp, reads=rb, writes=rb)

                    def r14():
                        L = cx.L
                        W = cx.W.t
                        rb = [cx.W.b]
                        le4 = L.t[:, :, 4:36].rearrange("p g (a j) -> p g a j", a=4)
                        P.op("dve", lambda e: e.tensor_reduce(W[:, :, 5:6], W[:, :, 16:20], AX.X, ALU.add), reads=rb, writes=rb)
                        P.op("dve", lambda e: e.reciprocal(W[:, :, 6:7], W[:, :, 5:6]), reads=rb, writes=rb)
                        P.ts("dve", W[:, :, 20:24], W[:, :, 8:12], 1.0, BIG, ALU.subtract, ALU.mult, reads=rb, writes=rb)
                        P.tt("dve", W[:, :, 32:64].rearrange("p g (a j) -> p g a j", a=4), le4,
                             W[:, :, 20:24].unsqueeze(3).to_broadcast([128, RG, 4, 8]), ALU.add, reads=[L.b] + rb, writes=rb)
                        P.op("dve", lambda e: e.tensor_reduce(W[:, :, 24:25], W[:, :, 32:64], AX.X, ALU.max), reads=rb, writes=rb)
                        P.tt("dve", W[:, :, 64:96], W[:, :, 32:64], W[:, :, 24:25].to_broadcast([128, RG, 32]), ALU.is_equal,
                             reads=rb, writes=rb)
                        P.stt("dve", W[:, :, 96:128], W[:, :, 64:96], -BIG, W[:, :, 32:64], ALU.mult, ALU.add, reads=rb, writes=rb)
                        P.op("dve", lambda e: e.tensor_reduce(W[:, :, 25:26], W[:, :, 96:128], AX.X, ALU.max), reads=rb, writes=rb)
                        P.tt("dve", W[:, :, 128:160], W[:, :, 96:128], W[:, :, 25:26].to_broadcast([128, RG, 32]), ALU.is_equal,
                             reads=rb, writes=rb)
                        P.tt("dve", W[:, :, 26:27], W[:, :, 25:26], W[:, :, 24:25], ALU.subtract, reads=rb, writes=rb)
                        P.act(W[:, :, 27:28], W[:, :, 26:27], AF.Exp, reads=rb, writes=rb)

                    def r15():
                        W = cx.W.t
                        rb = [cx.W.b]
                        P.ts("dve", W[:, :, 0:1], W[:, :, 27:28], 1.0, None, ALU.add, reads=rb, writes=rb)
                        P.op("dve", lambda e: e.reciprocal(W[:, :, 1:2], W[:, :, 0:1]), reads=rb, writes=rb)
                        P.tt("dve", W[:, :, 2:3], W[:, :, 1:2], W[:, :, 6:7], ALU.mult, reads=rb, writes=rb)
                        P.tt("dve", W[:, :, 3:4], W[:, :, 2:3], W[:, :, 27:28], ALU.mult, reads=rb, writes=rb)
                        P.tt("dve", W[:, :, 32:64], W[:, :, 64:96], W[:, :, 2:3].to_broadcast([128, RG, 32]), ALU.mult,
                             reads=rb, writes=rb)
                        P.tt("dve", W[:, :, 96:128], W[:, :, 128:160], W[:, :, 3:4].to_broadcast([128, RG, 32]), ALU.mult,
                             reads=rb, writes=rb)
                        P.tt("dve", gates.t[:, t - RG + 1:t + 1, :], W[:, :, 32:64], W[:, :, 96:128], ALU.add,
                             reads=rb, writes=[gates.b])

                    if t % RG != RG - 1:
                        r13 = r14 = r15 = None
                    return [s0, s1, s2, s3, s4, s5, s6, s7, s8, s9, s10, s10b, s11, s12, r13, r14, r15]

                tilesB = [b_tile(t) for t in range(NT_OWN)]
                run_pipeline(tilesB, [7, 12, 11, 10, 9, 8, 6, 5, 4, 3, 2, 1, 0, 16, 15, 14, 13])
                with nc.Block() as block:
                    P.emit_phase(block)
        if _STOP == 2:
            return nc
        wes.close()

        with ExitStack() as es:
            def sb(name, shape, dt):
                return TB(es.enter_context(nc.sbuf_tensor("sb_" + name, list(shape), dt)), name)

            def ps(name, shape, dt=F32):
                return TB(es.enter_context(nc.psum_tensor("ps_" + name, list(shape), dt)), name)

            NSUB = TPB // 4 if TPB >= 4 else 1
            TPS = TPB // NSUB
            h2sub = [sb("h2blk%d" % i, [128, KC, TPS * 128], BF16) for i in range(NSUB)]
            dh2 = [new_dsem() for _ in range(NSUB)]
            yacc = [sb("yacc%d" % i, [128, D], F32) for i in range(TPB)]
            wi_r = Ring([sb("wi%d" % i, [128, KC, 2 * DEXP], BF16) for i in range(2)])
            wo_r = Ring([sb("wo%d" % i, [128, 2, D], BF16) for i in range(2)])
            we_d = [new_dsem(), new_dsem()]
            sa_r = Ring([sb("sa%d" % i, [128, 512], F32) for i in range(2)])
            gT_r = Ring([sb("gT%d" % i, [128, 2, 512], BF16) for i in range(3)])
            x1t_r = Ring([sb("x1t%d" % i, [128, D], F32) for i in range(5)])
            x1t_d = [new_dsem() for _ in range(5)]
            sqj2 = sb("sqj2", [128, D], BF16)
            st2_r = Ring([sb("st2%d" % i, [128, 4], F32) for i in range(4)])
            yo_r = Ring([sb("yo%d" % i, [128, D], F32) for i in range(3)])
            yo_d = [new_dsem() for _ in range(3)]
            pa = [ps("pa%d" % q, [128, 512]) for q in range(2)]
            pu = [ps("pu%d" % q, [128, 512]) for q in range(2)]
            po_r = Ring([(ps("po%da" % i, [128, 512]), ps("po%db" % i, [128, 512])) for i in range(2)])
            out_dsems = yo_d

            def load_h2(blk, sub):
                tok0 = (blk * TPB + sub * TPS) * 128
                P.dma("sp", h2sub[sub].t[:], h2_scr[:, :, tok0:tok0 + TPS * 128], dh2[sub],
                      reads=[h2_b[blk * TPB + sub * TPS + i] for i in range(TPS)], writes=[h2sub[sub].b])

            fin_tiles = []

            NFS = 4

            def fin_stage(ft):
                stage, blk, ti, cx = ft
                tg = blk * TPB + ti
                if stage == 0:
                    x1t = cx["x1t"] = x1t_r.next()
                    P.dma("sp", x1t.t[:], y[tg * 128:(tg + 1) * 128, :], x1t_d[x1t_r.i], reads=[y_b[tg]], writes=[x1t.b])
                elif stage == 1:
                    x1t = cx["x1t"]
                    P.tt("pool", x1t.t[:], x1t.t[:], yacc[ti].t[:], ALU.add, reads=[x1t.b, yacc[ti].b], writes=[x1t.b])
                elif stage == 2:
                    x1t = cx["x1t"]
                    st = cx["st"] = st2_r.next()
                    P.act(sqj2.t[:], x1t.t[:], AF.Square, accum_out=st.t[:, 0:1], reads=[x1t.b], writes=[st.b])
                    P.act(st.t[:, 1:2], st.t[:, 0:1], AF.Ln, bias=EPS, scale=1.0 / D, reads=[st.b], writes=[st.b])
                    P.act(st.t[:, 2:3], st.t[:, 1:2], AF.Exp, scale=-0.5, reads=[st.b], writes=[st.b])
                else:
                    x1t, st = cx["x1t"], cx["st"]
                    yo = yo_r.next()
                    P.stt("dve", yo.t[:], x1t.t[:], st.t[:, 2:3], fgB.t[:], ALU.mult, ALU.mult,
                          reads=[x1t.b, st.b, fgB.b], writes=[yo.b])
                    P.dma("sp", y[tg * 128:(tg + 1) * 128, :], yo.t[:], yo_d[yo_r.i], reads=[yo.b], writes=[y_b[tg]])
                ft[0] = stage + 1

            def fin_step():
                todo = [ft for ft in fin_tiles if ft[0] < NFS][:NFS]
                for ft in todo:
                    fin_stage(ft)
                return len(todo) > 0

            def fin_require(blk, ti):
                for ft in fin_tiles:
                    if ft[1] == blk and ft[2] == ti:
                        while ft[0] < 2:
                            fin_step()

            def moe_down(blk, e_, sub, gT, wo, part=None):
                hs = max(1, TPS // 2)
                jr = range(TPS) if part is None else (range(0, hs) if part == 0 else range(hs, TPS))
                last_part = part is None or part == 1
                for j in jr:
                    ti = sub * TPS + j
                    tg = blk * TPB + ti
                    po = po_r.next()
                    for half in range(2):
                        for q in range(2):
                            P.mm(po[half].t[:, :], gT.t[:, q, j * 128:(j + 1) * 128],
                                 wo.t[:, q, half * 512:(half + 1) * 512], start=(q == 0), stop=(q == 1),
                                 reads=[gT.b, wo.b], writes=[po[half].b])
                    if e_ == 0 and blk > 0:
                        fin_require(blk - 1, ti)
                    for half in range(2):
                        ya = yacc[ti].t[:, half * 512:(half + 1) * 512]
                        if e_ == 0:
                            P.ts("dve", ya, po[half].t[:, :], gates.t[:, tg, e_:e_ + 1], None, ALU.mult,
                                 reads=[po[half].b, gates.b], writes=[yacc[ti].b])
                        else:
                            P.stt("dve", ya, po[half].t[:, :], gates.t[:, tg, e_:e_ + 1], ya, ALU.mult, ALU.add,
                                  reads=[po[half].b, gates.b, yacc[ti].b], writes=[yacc[ti].b])
                if not last_part:
                    return
                if e_ == NEXP - 1:
                    for j in range(TPS):
                        fin_tiles.append([0, blk, sub * TPS + j, {}])
                for _ in range(4 if blk == NBLK - 1 else 2):
                    fin_step()

            pending = None
            for sub in range(NSUB):
                load_h2(0, sub)
            seq = [(b_, e_) for b_ in range(NBLK) for e_ in range(NEXP)]

            def issue_w(idx):
                e_ = seq[idx][1]
                wi = wi_r.next()
                wo = wo_r.next()
                P.dma("pool", wi.t[:], w_ei[e_].rearrange("(k p) c -> p k c", p=128), we_d[wi_r.i], writes=[wi.b])
                P.dma("pool", wo.t[:], w_eo[e_].rearrange("(k p) c -> p k c", p=128), we_d[wi_r.i], writes=[wo.b],
                      batch=True)
                P.tt("pool", wo.t[:], wo.t[:], gtB.t[:, 1, :].unsqueeze(1).to_broadcast([128, 2, D]), ALU.mult,
                     reads=[wo.b, gtB.b], writes=[wo.b])
                return wi, wo

            wcur = issue_w(0)
            if True:
                for idx, (blk, e_) in enumerate(seq):
                    wi, wo = wcur
                    for sub in range(NSUB):
                        ns = TPS * 128
                        hb = h2sub[sub]
                        gT = gT_r.next()
                        for q in range(2):
                            for kc in range(KC):
                                P.mm(pa[q].t[:, 0:ns], wi.t[:, kc, q * 128:(q + 1) * 128], hb.t[:, kc, :],
                                     start=(kc == 0), stop=(kc == KC - 1), reads=[wi.b, hb.b], writes=[pa[q].b])
                            for kc in range(KC):
                                P.mm(pu[q].t[:, 0:ns], wi.t[:, kc, DEXP + q * 128:DEXP + (q + 1) * 128],
                                     hb.t[:, kc, :], start=(kc == 0), stop=(kc == KC - 1),
                                     reads=[wi.b, hb.b], writes=[pu[q].b])
                            sa = sa_r.next()
                            P.act(sa.t[:, 0:ns], pa[q].t[:, 0:ns], AF.Silu, reads=[pa[q].b], writes=[sa.b])
                            P.tt("dve", gT.t[:, q, 0:ns], sa.t[:, 0:ns], pu[q].t[:, 0:ns], ALU.mult,
                                 reads=[sa.b, pu[q].b], writes=[gT.b])
                            if pending is not None:
                                moe_down(*pending, part=q)
                        if e_ == NEXP - 1 and blk + 1 < NBLK:
                            load_h2(blk + 1, sub)
                        pending = (blk, e_, sub, gT, wo)
                        if sub == 0 and idx + 1 < len(seq):
                            wcur = issue_w(idx + 1)
            if pending is not None:
                moe_down(*pending)
                pending = None
            while fin_step():
                pass
            with nc.Block() as block:
                P.emit_phase(block, final=out_dsems)
    return nc


def _win_matrix(n, w):
    lo = w // 2
    hi = w - 1 - lo
    M = np.zeros((n, n), np.float32)
    for i in range(n):
        s = min(max(i - lo, 0), n)
        e = min(max(i + hi + 1, 0), n)
        M[i, s:e] = 1.0 / float(e - s)
    return M


def _pool_blocks(rows_total, par):
    blocks = np.zeros((NBLK_A, 128, 128), np.float32)
    eye = np.eye(128, dtype=np.float32)
    for gi, (w, R) in enumerate(zip(POOL_W, POOL_R)):
        Mr = _win_matrix(rows_total, w)
        Mc = _win_matrix(GRID_W, w)
        if par:
            Mr = Mr[::-1, ::-1]
            Mc = Mc[::-1, ::-1]
        Acol = np.ascontiguousarray(Mc.T)
        for v in range(R + 1):
            t = v
            for dt in range(-R, R + 1):
                if (v, gi, dt) not in POOL_TAB:
                    continue
                blk = np.zeros((2, GRID_W, 2, GRID_W), np.float32)
                for a in range(2):
                    r_in = 2 * (t + dt) + a
                    for b2 in range(2):
                        r_out = 2 * t + b2
                        if r_in < rows_total and r_out < rows_total:
                            blk[a, :, b2, :] = Mr[r_out, r_in] * Acol
                blk = blk.reshape(128, 128)
                if dt == 0:
                    blk = blk - eye
                blocks[POOL_TAB[(v, gi, dt)]] = blk
    return blocks


_PROG_CACHE = {}


def kernel(x, c, ctx, c_ctx, w_ada, b_ada, norm1_g, w_in, w_decay, b_decay, gla_norm_g, w_pool, pool_scale,
           w_out, norm2_g, w_router_group, w_router_expert, w_expert_in, w_expert_out, final_norm_g):
    f = lambda a: np.ascontiguousarray(np.asarray(a, dtype=np.float32))
    x, c, ctx, c_ctx = f(x), f(c), f(ctx), f(c_ctx)
    Bsz, SEQ, _ = x.shape
    NT = SEQ // 128
    NT_OWN = NT // 2
    NT_OTH = NT - NT_OWN
    NCTX = ctx.shape[1] // 128
    rows_total = SEQ // GRID_W
    n_cores = 2 * Bsz
    key = (NT_OWN, NT_OTH, NCTX)
    if key not in _PROG_CACHE:
        _PROG_CACHE[key] = build_program(*key)
    nc = _PROG_CACHE[key]

    w_ada0, b_ada0 = f(w_ada)[0], f(b_ada)[0]
    w_in0 = f(w_in)[0]
    w_dec0, b_dec0 = f(w_decay)[0], f(b_decay)[0]
    tri_f = np.triu(np.ones((128, 128), np.float32))
    cmat = np.stack([np.eye(128, dtype=np.float32), tri_f, np.ascontiguousarray(tri_f.T)])
    common = {
        "w_ada": w_ada0,
        "b_adaT": np.ascontiguousarray(b_ada0.reshape(48, 128).T),
        "b_gtB": np.ascontiguousarray(np.broadcast_to(
            np.stack([b_ada0[2 * D:3 * D], b_ada0[5 * D:6 * D]])[None], (128, 2, D))),
        "g12T": np.ascontiguousarray(np.stack([f(norm1_g)[0].reshape(KC, 128).T, f(norm2_g)[0].reshape(KC, 128).T], axis=1)),
        "ggB": np.ascontiguousarray(np.broadcast_to(f(gla_norm_g)[0][None, :], (128, 128))),
        "w_pool": f(w_pool)[0],
        "pscT": np.ascontiguousarray(f(pool_scale)[0].reshape(4, 128).T),
        "w_out": f(w_out)[0],
        "w_r": np.ascontiguousarray(np.concatenate([f(w_router_group)[0], f(w_router_expert)[0]], axis=1)),
        "w_ei": f(w_expert_in)[0],
        "w_eo": f(w_expert_out)[0],
        "fgB": np.ascontiguousarray(np.broadcast_to(f(final_norm_g)[None, :], (128, D))),
        "cmat": cmat,
    }
    per_par = []
    for par in range(2):
        d0, d1 = (0, 1) if par == 0 else (1, 0)
        wi = w_in0.copy()
        if par:
            wi[:, OFF_A:OFF_A + 16] = w_in0[:, OFF_A + 16:OFF_A + 32]
            wi[:, OFF_A + 16:OFF_A + 32] = w_in0[:, OFF_A:OFF_A + 16]
        wdec = np.zeros((33, 512), np.float32)
        wdec[0:16, 0:256] = w_dec0[d0]
        wdec[16:32, 256:512] = w_dec0[d1]
        wdec[32, 0:256] = b_dec0[d0]
        wdec[32, 256:512] = b_dec0[d1]
        per_par.append({"w_in": wi, "wdec": wdec, "apool": _pool_blocks(rows_total, par)})
    in_maps = []
    for core in range(n_cores):
        b, par = core // 2, core % 2
        m = dict(common)
        m.update(per_par[par])
        xb, cb = x[b], ctx[b]
        if par:
            xb, cb = xb[::-1], cb[::-1]
        m["xs"] = np.ascontiguousarray(xb)
        m["ctxs"] = np.ascontiguousarray(cb)
        m["cvec"] = np.ascontiguousarray(np.stack([c[b].reshape(KC, 128).T, c_ctx.reshape(KC, 128).T], axis=2))
        in_maps.append(m)
    res = run_bass_kernel_spmd(nc, in_maps, core_ids=list(range(n_cores)))
    out = np.empty((Bsz, SEQ, D), np.float32)
    half = NT_OWN * 128
    for core in range(n_cores):
        b, par = core // 2, core % 2
        yv = res.results[core]["y"]
        if par == 0:
            out[b, :half] = yv
        else:
            out[b, half:] = yv[::-1]
    return out
```
